# Optimizing a Trainium2 kernel written in Bass

```python
import math, functools
import jax, jax.numpy as jnp
from jax import lax
import numpy as np

D_MODEL = 2048
BATCH = 8
SEQ = 4096
DEPTH = 2

HEAD_DIM = 128
N_MIX_HEADS = D_MODEL // HEAD_DIM
SB_HEADS = N_MIX_HEADS // 2
GDN_HEADS = N_MIX_HEADS - SB_HEADS
MOBA_HEADS = N_MIX_HEADS // 2
DIL_HEADS = N_MIX_HEADS - MOBA_HEADS
SB_W = SB_HEADS * HEAD_DIM
GDN_W = GDN_HEADS * HEAD_DIM
MOBA_W = MOBA_HEADS * HEAD_DIM
DIL_W = DIL_HEADS * HEAD_DIM
MIX_W_EVEN = SB_W + GDN_W
MIX_W_ODD = MOBA_W + DIL_W
SB_BLOCK = 128
GDN_CHUNK = 64
GDN_CONV = 4
MOBA_BLOCK = 256
MOBA_TOPK = 3
MOBA_QCHUNK = 32
DIL_WINDOWS = (128, 512, 2048)
DIL_RATES = (1, 4, 16)
D_FF = ((8 * D_MODEL + 3 * 256 - 1) // (3 * 256)) * 256
NORM_EPS = 1e-6
N_EVEN = (DEPTH + 1) // 2
N_ODD = DEPTH // 2
EVEN_SIZES = (SB_W, SB_W, SB_W, 3 * GDN_W, GDN_W, GDN_HEADS, GDN_HEADS)
EVEN_IN = sum(EVEN_SIZES)
ODD_SIZES = (MOBA_W, MOBA_W, MOBA_W, DIL_W, DIL_W, DIL_W)
ODD_IN = sum(ODD_SIZES)

kernel_name = "hybrid_stickbreak_gdn_moba_dilated"


def _split_points(sizes):
    return list(np.cumsum(sizes)[:-1])


def rms_norm(x, w):
    xf = x.astype(jnp.float32)
    y = xf * lax.rsqrt(jnp.mean(xf * xf, axis=-1, keepdims=True) + NORM_EPS)
    return (y * w.astype(jnp.float32)).astype(x.dtype)


def l2norm(t):
    return t * lax.rsqrt(jnp.sum(t * t, axis=-1, keepdims=True) + NORM_EPS)


def to_heads(t, n):
    b, s, _ = t.shape
    return t.reshape(b, s, n, -1).transpose(0, 2, 1, 3)


def from_heads(t):
    b, h, s, d = t.shape
    return t.transpose(0, 2, 1, 3).reshape(b, s, h * d)


def causal_depthwise_conv(x, w):
    k, c = w.shape
    return lax.conv_general_dilated(
        x, w[:, None, :].astype(x.dtype), window_strides=(1,), padding=[(k - 1, 0)],
        dimension_numbers=("NWC", "WIO", "NWC"), feature_group_count=c)


def stick_breaking_attention(q, k, v):
    b, h, s, d = q.shape
    nq = s // SB_BLOCK
    scale = d ** -0.5
    qb = q.reshape(b, h, nq, SB_BLOCK, d).transpose(2, 0, 1, 3, 4)
    kpos = jnp.arange(s)

    def block(args):
        qc, i = args
        z = jnp.einsum("bhqd,bhkd->bhqk", qc, k).astype(jnp.float32) * scale
        qpos = i * SB_BLOCK + jnp.arange(SB_BLOCK)
        past = kpos[None, :] < qpos[:, None]
        log_1m = jnp.where(past, jax.nn.log_sigmoid(-z), 0.0)
        between = lax.cumsum(log_1m, axis=3, reverse=True) - log_1m
        wts = jnp.where(past, jnp.exp(jax.nn.log_sigmoid(z) + between), 0.0)
        return jnp.einsum("bhqk,bhkd->bhqd", wts.astype(v.dtype), v)

    out = lax.map(block, (qb, jnp.arange(nq)))
    return out.transpose(1, 2, 0, 3, 4).reshape(b, h, s, d)


def gated_delta_rule(q, k, v, g, beta):
    b, h, s, dk = q.shape
    dv = v.shape[-1]
    c = GDN_CHUNK
    n = s // c
    f32 = jnp.float32
    q = l2norm(q.astype(f32)) * dk ** -0.5
    k = l2norm(k.astype(f32))
    v = v.astype(f32)
    chunk = lambda t: t.reshape((b, h, n, c) + t.shape[3:])
    q, k, v, g, beta = map(chunk, (q, k, v, g.astype(f32), beta.astype(f32)))
    g = jnp.cumsum(g, axis=-1)
    incl = jnp.tril(jnp.ones((c, c), bool))
    strict = jnp.tril(jnp.ones((c, c), bool), -1)
    diff = g[..., :, None] - g[..., None, :]
    decay = jnp.where(incl, jnp.exp(jnp.where(incl, diff, 0.0)), 0.0)
    kb = k * beta[..., None]
    lower = jnp.where(strict, jnp.einsum("bhncd,bhnkd->bhnck", kb, k) * decay, 0.0)
    tmat = lower + jnp.eye(c, dtype=f32)
    solve = functools.partial(lax.linalg.triangular_solve, left_side=True, lower=True,
                              unit_diagonal=True)
    u = solve(tmat, v * beta[..., None])
    w = solve(tmat, kb * jnp.exp(g)[..., None])
    intra = jnp.where(incl, jnp.einsum("bhncd,bhnkd->bhnck", q, k) * decay, 0.0)
    qg = q * jnp.exp(g)[..., None]
    g_last = g[..., -1]
    kd = k * jnp.exp(g_last[..., None] - g)[..., None]

    def step(state, inp):
        qg_c, kd_c, u_c, w_c, intra_c, gl_c = inp
        v_new = u_c - jnp.einsum("bhcd,bhde->bhce", w_c, state)
        o = (jnp.einsum("bhcd,bhde->bhce", qg_c, state)
             + jnp.einsum("bhck,bhke->bhce", intra_c, v_new))
        state = state * jnp.exp(gl_c)[..., None, None] + jnp.einsum("bhcd,bhce->bhde", kd_c, v_new)
        return state, o

    xs = tuple(jnp.moveaxis(t, 2, 0) for t in (qg, kd, u, w, intra, g_last))
    _, o = lax.scan(step, jnp.zeros((b, h, dk, dv), f32), xs)
    return jnp.moveaxis(o, 0, 2).reshape(b, h, s, dv)


def moba_attention(q, k, v):
    b, h, s, d = q.shape
    bs = MOBA_BLOCK
    sp = -(-s // bs) * bs
    pad = ((0, 0), (0, 0), (0, sp - s), (0, 0))
    q, k, v = (jnp.pad(t, pad) for t in (q, k, v))
    nb = sp // bs
    ksel = min(MOBA_TOPK, nb - 1)
    scale = d ** -0.5
    kb = k.reshape(b, h, nb, bs, d)
    vb = v.reshape(b, h, nb, bs, d)
    own = jnp.arange(sp) // bs
    if ksel > 0:
        kmean = jnp.mean(kb.astype(jnp.float32), axis=3)
        gate = jnp.einsum("bhsd,bhnd->bhsn", q.astype(jnp.float32), kmean)
        fully_past = jnp.arange(nb)[None, :] < own[:, None]
        gate = jnp.where(fully_past, gate, -jnp.inf)
        _, sel = lax.top_k(gate, ksel)
    else:
        sel = jnp.zeros((b, h, sp, 0), jnp.int32)
    valid = sel < own[None, None, :, None]
    nq = sp // MOBA_QCHUNK
    qs = jnp.moveaxis(q.reshape(b, h, nq, MOBA_QCHUNK, d), 2, 0)
    sels = jnp.moveaxis(sel.reshape(b, h, nq, MOBA_QCHUNK, ksel), 2, 0)
    vals = jnp.moveaxis(valid.reshape(b, h, nq, MOBA_QCHUNK, ksel), 2, 0)
    bi = jnp.arange(b)[:, None, None]
    hi = jnp.arange(h)[None, :, None]

    def chunk(args):
        qc, selc, valc, ci = args
        ob = (ci * MOBA_QCHUNK) // bs
        k_own = lax.dynamic_index_in_dim(kb, ob, axis=2, keepdims=False)
        v_own = lax.dynamic_index_in_dim(vb, ob, axis=2, keepdims=False)
        qpos = ci * MOBA_QCHUNK + jnp.arange(MOBA_QCHUNK)
        kpos = ob * bs + jnp.arange(bs)
        s_own = jnp.einsum("bhqd,bhkd->bhqk", qc, k_own).astype(jnp.float32) * scale
        s_own = jnp.where(kpos[None, :] <= qpos[:, None], s_own, -jnp.inf)
        scores, v_sel = [], []
        for j in range(ksel):
            kj = kb[bi, hi, selc[..., j]]
            v_sel.append(vb[bi, hi, selc[..., j]])
            sj = jnp.einsum("bhqd,bhqkd->bhqk", qc, kj).astype(jnp.float32) * scale
            scores.append(jnp.where(valc[..., j, None], sj, -jnp.inf))
        p = jax.nn.softmax(jnp.concatenate(scores + [s_own], axis=-1), axis=-1).astype(v.dtype)
        o = jnp.einsum("bhqk,bhkd->bhqd", p[..., ksel * bs:], v_own)
        for j in range(ksel):
            o = o + jnp.einsum("bhqk,bhqkd->bhqd", p[..., j * bs:(j + 1) * bs], v_sel[j])
        return o

    out = lax.map(chunk, (qs, sels, vals, jnp.arange(nq)))
    return jnp.moveaxis(out, 0, 2).reshape(b, h, sp, d)[:, :, :s]


def dilated_branch(q, k, v, window, rate):
    b, h, s, d = q.shape
    span = window // rate
    seg = rate * span
    sp = -(-s // seg) * seg
    nblk = sp // seg
    scale = d ** -0.5

    def to_residue(t):
        t = jnp.pad(t, ((0, 0), (0, 0), (0, sp - s), (0, 0)))
        t = t.reshape(b, h, sp // rate, rate, d).transpose(0, 1, 3, 2, 4)
        return t.reshape(b, h, rate, nblk, span, d)

    qr, kr, vr = to_residue(q), to_residue(k), to_residue(v)
    prev = lambda t: jnp.pad(t, ((0, 0),) * 3 + ((1, 0), (0, 0), (0, 0)))[:, :, :, :-1]
    kk = jnp.concatenate([prev(kr), kr], axis=4)
    vv = jnp.concatenate([prev(vr), vr], axis=4)
    sc = jnp.einsum("bhrnqd,bhrnkd->bhrnqk", qr, kk).astype(jnp.float32) * scale
    qi = jnp.arange(span)[:, None]
    ki = jnp.arange(2 * span)[None, :]
    dist = span + qi - ki
    blk = jnp.arange(nblk)[:, None, None]
    ok = (dist >= 0) & (dist <= span) & ((blk > 0) | (ki >= span))[...]
    sc = jnp.where(ok, sc, -jnp.inf)
    m = jnp.max(sc, axis=-1, keepdims=True)
    p = jnp.exp(sc - m)
    den = jnp.sum(p, axis=-1)
    o = jnp.einsum("bhrnqk,bhrnkd->bhrnqd", p, vv.astype(jnp.float32)) / den[..., None]
    lse = m[..., 0] + jnp.log(den)
    o = o.reshape(b, h, rate, sp // rate, d).transpose(0, 1, 3, 2, 4).reshape(b, h, sp, d)[:, :, :s]
    lse = lse.reshape(b, h, rate, sp // rate).transpose(0, 1, 3, 2).reshape(b, h, sp)[:, :, :s]
    return o, lse


def dilated_attention(q, k, v):
    outs, lses = [], []
    for window, rate in zip(DIL_WINDOWS, DIL_RATES):
        o, lse = dilated_branch(q, k, v, window, rate)
        outs.append(o)
        lses.append(lse)
    wts = jax.nn.softmax(jnp.stack(lses, 0), axis=0)
    return jnp.einsum("gbhs,gbhsd->bhsd", wts, jnp.stack(outs, 0))


def even_mixer(u, w_in, conv_w, a_log, dt_bias, onorm_w, w_out):
    proj = u @ w_in
    sb_q, sb_k, sb_v, gdn_qkv, gdn_z, gdn_a, gdn_b = jnp.split(proj, _split_points(EVEN_SIZES), axis=-1)
    o_sb = stick_breaking_attention(to_heads(sb_q, SB_HEADS), to_heads(sb_k, SB_HEADS),
                                    to_heads(sb_v, SB_HEADS))
    qkv = jax.nn.silu(causal_depthwise_conv(gdn_qkv, conv_w))
    g_q, g_k, g_v = jnp.split(qkv, 3, axis=-1)
    f32 = jnp.float32
    beta = jax.nn.sigmoid(gdn_b.astype(f32)).transpose(0, 2, 1)
    g = (-jnp.exp(a_log.astype(f32)) * jax.nn.softplus(gdn_a.astype(f32) + dt_bias.astype(f32))
         ).transpose(0, 2, 1)
    o_gdn = gated_delta_rule(to_heads(g_q, GDN_HEADS), to_heads(g_k, GDN_HEADS),
                             to_heads(g_v, GDN_HEADS), g, beta)
    o_gdn = rms_norm(o_gdn, onorm_w) * jax.nn.silu(to_heads(gdn_z, GDN_HEADS).astype(f32))
    mixed = jnp.concatenate([from_heads(o_sb), from_heads(o_gdn).astype(u.dtype)], axis=-1)
    return mixed @ w_out


def odd_mixer(u, w_in, w_out):
    proj = u @ w_in
    mq, mk, mv, dq, dk, dv = jnp.split(proj, _split_points(ODD_SIZES), axis=-1)
    o_moba = moba_attention(to_heads(mq, MOBA_HEADS), to_heads(mk, MOBA_HEADS), to_heads(mv, MOBA_HEADS))
    o_dil = dilated_attention(to_heads(dq, DIL_HEADS), to_heads(dk, DIL_HEADS), to_heads(dv, DIL_HEADS))
    mixed = jnp.concatenate([from_heads(o_moba), from_heads(o_dil).astype(u.dtype)], axis=-1)
    return mixed @ w_out


def swiglu(u, w_gate, w_up, w_down):
    return (jax.nn.silu(u @ w_gate) * (u @ w_up)) @ w_down


def setup_inputs(seed: int = 0) -> dict:
    key = jax.random.key(seed)
    ks = jax.random.split(key, 20)
    f32 = jnp.float32
    nrm = lambda k, shape, sc: jax.random.normal(k, shape, f32) * sc
    gain = lambda k, shape: 1.0 + 0.02 * jax.random.normal(k, shape, f32)
    x = nrm(ks[0], (BATCH, SEQ, D_MODEL), 1.0)
    dt = jnp.exp(jax.random.uniform(ks[8], (N_EVEN, GDN_HEADS), f32, math.log(1e-3), math.log(1e-1)))
    return {
        "x": x,
        "mix_norm_pre": gain(ks[1], (DEPTH, D_MODEL)),
        "mix_norm_post": gain(ks[2], (DEPTH, D_MODEL)),
        "ffn_norm_pre": gain(ks[3], (DEPTH, D_MODEL)),
        "ffn_norm_post": gain(ks[4], (DEPTH, D_MODEL)),
        "ev_w_in": nrm(ks[5], (N_EVEN, D_MODEL, EVEN_IN), D_MODEL ** -0.5),
        "ev_conv_w": nrm(ks[6], (N_EVEN, GDN_CONV, 3 * GDN_W), GDN_CONV ** -0.5),
        "ev_a_log": jnp.log(jax.random.uniform(ks[7], (N_EVEN, GDN_HEADS), f32, 1.0, 16.0)),
        "ev_dt_bias": dt + jnp.log(-jnp.expm1(-dt)),
        "ev_onorm": gain(ks[9], (N_EVEN, HEAD_DIM)),
        "ev_w_out": nrm(ks[10], (N_EVEN, MIX_W_EVEN, D_MODEL), MIX_W_EVEN ** -0.5),
        "od_w_in": nrm(ks[11], (N_ODD, D_MODEL, ODD_IN), D_MODEL ** -0.5),
        "od_w_out": nrm(ks[12], (N_ODD, MIX_W_ODD, D_MODEL), MIX_W_ODD ** -0.5),
        "ffn_w_gate": nrm(ks[13], (DEPTH, D_MODEL, D_FF), D_MODEL ** -0.5),
        "ffn_w_up": nrm(ks[14], (DEPTH, D_MODEL, D_FF), D_MODEL ** -0.5),
        "ffn_w_down": nrm(ks[15], (DEPTH, D_FF, D_MODEL), D_FF ** -0.5),
    }


def reference(x, mix_norm_pre, mix_norm_post, ffn_norm_pre, ffn_norm_post, ev_w_in, ev_conv_w,
              ev_a_log, ev_dt_bias, ev_onorm, ev_w_out, od_w_in, od_w_out, ffn_w_gate, ffn_w_up,
              ffn_w_down):
    h = x
    for layer in range(DEPTH):
        i = layer // 2
        u = rms_norm(h, mix_norm_pre[layer])
        if layer % 2 == 0:
            u = even_mixer(u, ev_w_in[i], ev_conv_w[i], ev_a_log[i], ev_dt_bias[i], ev_onorm[i], ev_w_out[i])
        else:
            u = odd_mixer(u, od_w_in[i], od_w_out[i])
        h = h + rms_norm(u, mix_norm_post[layer])
        u = swiglu(rms_norm(h, ffn_norm_pre[layer]), ffn_w_gate[layer], ffn_w_up[layer], ffn_w_down[layer])
        h = h + rms_norm(u, ffn_norm_post[layer])
    return h
```

```python
from contextlib import ExitStack
import numpy as np
import concourse.bass as bass
import concourse.mybir as mybir
from concourse.bass_utils import run_bass_kernel_spmd

F32 = mybir.dt.float32
BF16 = mybir.dt.bfloat16
F32R = mybir.dt.float32r
AF = mybir.ActivationFunctionType
ALU = mybir.AluOpType
AX = mybir.AxisListType

D = 2048
FF = 5632
HD = 128
EPS = 1e-6
EVEN_IN = 7184
ODD_IN = 6144
SCALE = HD ** -0.5
NEG = -30000.0


class Buf:
    __slots__ = ("t", "w", "r", "name", "dsem")

    def __init__(self, t, name):
        self.t = t
        self.w = {}
        self.r = {}
        self.name = name
        self.dsem = None

    def __getitem__(self, idx):
        return self.t[idx]


class KB:
    def __init__(self, nc):
        self.nc = nc
        self.st = {}
        self.sems = {}
        for n in ["tensor", "vector", "scalar", "gpsimd", "sync"]:
            h = nc.alloc_semaphore("s_" + n)
            self.sems[h.num] = [h, 0, False]
            self.st[n] = dict(e=getattr(nc, n), sem=h, seen={})
        self.nbuf = 0
        self.nwait = 0
        self.nop = 0
        self.dsem_pool = {"sw": [], "hw": []}
        self.es = None

    def _nm(self, p):
        self.nbuf += 1
        return "%s%d" % (p, self.nbuf)

    def sb(self, shape, dt, name=None):
        name = self._nm(name or "b")
        t = self.es.enter_context(self.nc.sbuf_tensor(name, list(shape), dt))
        return Buf(t, name)

    def ps(self, shape, dt=F32, name=None):
        name = self._nm(name or "p")
        t = self.es.enter_context(self.nc.psum_tensor(name, list(shape), dt))
        return Buf(t, name)

    def dram(self, name, shape, dt, kind="Internal"):
        t = self.nc.dram_tensor(name, list(shape), dt, kind=kind)
        return Buf(t.ap(), name)

    def _waits(self, eng, reads, writes, pwrites):
        st = self.st[eng]
        own = st["sem"].num
        skip_own = eng == "tensor"
        need = {}
        for b in reads:
            for s, v in b.w.items():
                if need.get(s, 0) < v:
                    need[s] = v
        for b in writes:
            for s, v in b.w.items():
                if need.get(s, 0) < v:
                    need[s] = v
            for s, v in b.r.items():
                if need.get(s, 0) < v:
                    need[s] = v
        for b in pwrites:
            for s, v in b.r.items():
                if need.get(s, 0) < v:
                    need[s] = v
        if skip_own:
            need.pop(own, None)
        seen = st["seen"]
        e = st["e"]
        for s, v in need.items():
            info = self.sems[s]
            if info[2]:
                v = info[1]
            if seen.get(s, 0) >= v:
                continue
            e.wait_ge(info[0], v)
            seen[s] = v
            self.nwait += 1

    def op(self, eng, fn, reads=(), writes=(), pwrites=()):
        self._waits(eng, reads, writes, pwrites)
        st = self.st[eng]
        ins = fn(st["e"])
        h = st["sem"]
        info = self.sems[h.num]
        info[1] += 1
        ins.then_inc(h, 1)
        c = info[1]
        for b in reads:
            b.r[h.num] = c
        for b in writes:
            b.w[h.num] = c
        for b in pwrites:
            b.w[h.num] = c
        self.nop += 1
        return ins

    def dma(self, q, out_ap, in_ap, reads=(), writes=(), pwrites=(), sem_of=None):
        self._waits(q, reads, writes, pwrites)
        b0 = sem_of
        kind = "sw" if q == "gpsimd" else "hw"
        if b0.dsem is None:
            if self.dsem_pool[kind]:
                h = self.dsem_pool[kind].pop()
            else:
                h = self.nc.alloc_semaphore(self._nm("d"))
                self.sems[h.num] = [h, 0, True]
            b0.dsem = h
            b0_kind = kind
            self.phase_dsems.append((kind, h))
        h = b0.dsem
        info = self.sems[h.num]
        info[1] += 16
        ins = self.st[q]["e"].dma_start(out=out_ap, in_=in_ap)
        ins.then_inc(h, 16)
        c = info[1]
        for b in reads:
            b.r[h.num] = c
        for b in writes:
            b.w[h.num] = c
        for b in pwrites:
            b.w[h.num] = c
        self.nop += 1
        return ins

    def barrier(self):
        for n, st in self.st.items():
            own = st["sem"].num
            for s, info in self.sems.items():
                if s == own or info[1] == 0:
                    continue
                if st["seen"].get(s, 0) >= info[1]:
                    continue
                st["e"].wait_ge(info[0], info[1])
                st["seen"][s] = info[1]
                self.nwait += 1

    def push_scope(self):
        self.es_stack = getattr(self, "es_stack", [])
        self.es_stack.append(self.es)
        self.es = ExitStack()

    def pop_scope(self):
        self.barrier()
        self.es.close()
        self.es = self.es_stack.pop()

    def begin_phase(self):
        self.es = ExitStack()
        self.phase_dsems = []

    def end_phase(self):
        self.barrier()
        self.es.close()
        self.es = None
        for kind, h in self.phase_dsems:
            self.dsem_pool[kind].append(h)
        self.phase_dsems = []


def make_consts(S):
    NT = S // 128
    c = {}
    c["ident"] = np.eye(128, dtype=np.float32)
    kp = np.arange(128)[:, None]
    qf = np.arange(512)[None, :]
    cm = np.zeros((128, 8, 512), np.float32)
    for r in range(4):
        cm[:, r, :] = (r * 128 + kp) < qf
        cm[:, 4 + r, :] = (r * 128 + kp) <= qf
    c["cmask"] = cm.reshape(128, 8 * 512)
    s = np.arange(128)[:, None]
    j = np.arange(128)[None, :]
    c["tri"] = (s > j).astype(np.float32)
    c["ones"] = np.ones((128, 128), np.float32)
    en = np.zeros((128, 16, 128), np.float32)
    for n in range(16):
        en[n, n, :] = 1.0
    c["esel"] = en.reshape(128, 16 * 128)
    gm = np.zeros((128, NT, 16), np.float32)
    om = np.zeros((128, NT, 16), np.float32)
    for t in range(NT):
        own = t // 2
        gm[:, t, own:] = -1e30
        if own < 16:
            om[:, t, own] = 1.0
    c["gmask"] = gm.reshape(128, NT * 16)
    c["ownmask"] = om.reshape(128, NT * 16)
    x = np.arange(2944)[None, :]
    dlt = x - 384 - kp
    m = ((dlt >= 0) & (dlt <= 128)).astype(np.float32)
    m += ((dlt >= 0) & (dlt <= 512) & (dlt % 4 == 0))
    m += ((dlt >= 0) & (dlt <= 2048) & (dlt % 16 == 0))
    c["mfull"] = m.astype(np.float32)
    sel8 = np.zeros((128, 8, 128), np.float32)
    for h in range(8):
        sel8[h, h, :] = 1.0
    c["sel8"] = sel8.reshape(128, 8 * 128)
    rm = np.ones((128, S), np.float32)
    rm[:, ::64] = 0.0
    c["scanmask"] = rm
    blk = (s // 64) == (j // 64)
    c["negstrict"] = np.where(blk & (s > j), 0.0, NEG).astype(np.float32)
    c["negincT"] = np.where(blk & (j >= s), 0.0, NEG).astype(np.float32)
    rowm = np.zeros((128, 2), np.float32)
    rowm[:64, 0] = 1.0
    rowm[64:, 1] = 1.0
    c["rowmask"] = rowm
    return c


CONST_SHAPES = lambda S: {k: v.shape for k, v in make_consts(S).items()}

WEIGHT_SPECS = [
    ("mix_norm_pre", (2, D)), ("mix_norm_post", (2, D)), ("ffn_norm_pre", (2, D)), ("ffn_norm_post", (2, D)),
    ("ev_w_in", (D, EVEN_IN)), ("ev_conv_w", (4, 3072)), ("ev_a_log", (1, 8)), ("ev_dt_bias", (1, 8)),
    ("ev_onorm", (1, 128)), ("ev_w_out", (D, D)), ("od_w_in", (D, ODD_IN)), ("od_w_out", (D, D)),
    ("ffn_w_gate", (2 * D, FF)), ("ffn_w_up", (2 * D, FF)), ("ffn_w_down", (2 * FF, D)),
]


class Prog:
    def __init__(self, S, io=None, declare_weights=True):
        self.S = S
        self.NT = S // 128
        self.T = min(1024, S)
        io = io or {}
        self.nc = bass.Bass("TRN2", target_bir_lowering=False)
        self.kb = KB(self.nc)
        kb = self.kb
        self.din = {}
        self.din["x"] = kb.dram("x", [S, D], F32, kind="ExternalInput")
        for n, shp in WEIGHT_SPECS:
            if declare_weights or shp[0] * shp[1] < 100000:
                self.din[n] = kb.dram(n, list(shp), F32, kind="ExternalInput")
        self.cst = {}
        for n, shp in CONST_SHAPES(S).items():
            self.cst[n] = kb.dram("c_" + n, list(shp), F32, kind="ExternalInput")

        def scratch(name, shape, dt):
            kind = io.get(name, "Internal")
            return kb.dram(name, shape, dt, kind=kind)

        self.FM = scratch("FM", [5120, S], BF16)
        self.TM = scratch("TM", [S, 2048], BF16)
        self.ABtm = scratch("ABtm", [S, 16], F32)
        self.AFM = scratch("AFM", [8, S], F32)
        self.MIX = scratch("MIX", [2048, S], BF16)
        self.Y = scratch("Y", [S, D], F32)
        self.AT = scratch("AT", [FF, S], BF16)
        self.H1 = scratch("H1", [S, D], F32)
        self.H2 = scratch("H2", [S, D], F32)
        self.H3 = scratch("H3", [S, D], F32)
        self.OUT = kb.dram("y", [S, D], F32, kind="ExternalOutput")
        self.evc = 0

    def evac(self, out_ap, in_ap, reads, writes=(), pwrites=(), scale=None):
        kb = self.kb
        self.evc += 1
        if self.evc % 2 == 0:
            if scale is None:
                kb.op("scalar", lambda e: e.activation(out=out_ap, in_=in_ap, func=AF.Copy),
                      reads=reads, writes=writes, pwrites=pwrites)
            else:
                kb.op("scalar", lambda e: e.activation(out=out_ap, in_=in_ap, func=AF.Copy, scale=scale),
                      reads=reads, writes=writes, pwrites=pwrites)
        else:
            if scale is None:
                kb.op("vector", lambda e: e.tensor_copy(out=out_ap, in_=in_ap),
                      reads=reads, writes=writes, pwrites=pwrites)
            else:
                kb.op("vector", lambda e: e.tensor_scalar(out=out_ap, in0=in_ap, scalar1=scale, scalar2=None,
                                                          op0=ALU.mult),
                      reads=reads, writes=writes, pwrites=pwrites)

    def load_const(self, name, shape, dt, src=None, q=None):
        kb = self.kb
        b = kb.sb(shape, dt, "c_" + name)
        src = src if src is not None else self.cst[name]
        if q is None:
            q = "gpsimd" if dt != F32 else "sync"
        kb.dma(q, b[:], src[:], reads=[src], writes=[b], sem_of=b)
        return b

    def load_gain(self, which, layer):
        kb = self.kb
        src = self.din[which]
        g = kb.sb([128, D], F32, "gain")
        kb.dma("sync", g[:], src.t[layer:layer + 1, :].to_broadcast([128, D]), reads=[src], writes=[g], sem_of=g)
        return g

    def rstd_from_ss(self, ss, n, mhalf, extra_scale=None):
        kb = self.kb
        kb.op("gpsimd", lambda e: e.tensor_scalar(out=ss[:, 1:2], in0=ss[:, 0:1], scalar1=1.0 / n, scalar2=EPS,
                                                  op0=ALU.mult, op1=ALU.add), reads=[ss], pwrites=[ss])
        kb.op("gpsimd", lambda e: e.tensor_tensor(out=ss[:, 1:2], in0=ss[:, 1:2], in1=mhalf[:, 0:1], op=ALU.pow),
              reads=[ss, mhalf], pwrites=[ss])

    def norm_transpose(self, hsrc, tok0, gain, uT, col0, env):
        kb = self.kb
        i = env["i"]
        env["i"] += 1
        xt = env["xt"][i % 2]
        ss = env["ss"][i % 2]
        yb = env["yb"][i % 2]
        pT = env["pT"]
        kb.dma("sync", xt[:], hsrc.t[tok0:tok0 + 128, :], reads=[hsrc], writes=[xt], sem_of=xt)
        kb.op("scalar", lambda e: e.activation(out=env["junk"][:], in_=xt[:], func=AF.Square,
                                               accum_out=ss[:, 0:1]),
              reads=[xt], writes=[env["junk"]], pwrites=[ss])
        self.rstd_from_ss(ss, D, env["mhalf"])
        kb.op("vector", lambda e: e.scalar_tensor_tensor(out=yb[:], in0=xt[:], scalar=ss[:, 1:2], in1=gain[:],
                                                         op0=ALU.mult, op1=ALU.mult),
              reads=[xt, ss, gain], writes=[yb])
        ident = env["identb"]
        for k in range(16):
            kb.op("tensor", lambda e, k=k: e.transpose(pT[:, k * 128:(k + 1) * 128], yb[:, k * 128:(k + 1) * 128],
                                                        ident[:]),
                  reads=[yb, ident], pwrites=[pT])
        for hf in range(2):
            self.evac(uT.t[:, hf * 8:(hf + 1) * 8, col0:col0 + 128],
                      pT.t[:, hf * 1024:(hf + 1) * 1024].rearrange("p (k c) -> p k c", c=128),
                      reads=[pT], pwrites=[uT])

    def norm_env(self):
        kb = self.kb
        env = dict(i=0)
        env["xt"] = [kb.sb([128, D], F32, "xt") for _ in range(2)]
        env["ss"] = [kb.sb([128, 2], F32, "ss") for _ in range(2)]
        env["yb"] = [kb.sb([128, D], BF16, "yb") for _ in range(2)]
        env["junk"] = kb.sb([128, D], BF16, "junk")
        env["pT"] = kb.ps([128, D], BF16, "pT")
        env["identb"] = self.load_const("ident", [128, 128], BF16)
        mh = kb.sb([128, 1], F32, "mhalf")
        kb.op("gpsimd", lambda e: e.memset(mh[:], -0.5), writes=[mh])
        env["mhalf"] = mh
        return env

    def phase_inproj(self, layer, hsrc):
        kb = self.kb
        S, T = self.S, self.T
        kb.begin_phase()
        env = self.norm_env()
        gain = self.load_gain("mix_norm_pre", layer)
        W = self.din["ev_w_in"] if layer == 0 else self.din["od_w_in"]
        if layer == 0:
            blocks = [(c, 512, "FM", c) for c in range(0, 2048, 512)]
            blocks += [(c, 512, "TM", c - 2048) for c in range(2048, 3072, 512)]
            blocks += [(c, 512, "FM", c - 1024) for c in range(3072, 6144, 512)]
            blocks += [(c, 512, "TM", c - 6144 + 1024) for c in range(6144, 7168, 512)]
            blocks += [(7168, 16, "AB", 0)]
        else:
            blocks = [(c, 512, "FM", c) for c in range(0, 2048, 512)]
            blocks += [(c, 512, "TM", c - 2048) for c in range(2048, 3072, 512)]
            blocks += [(c, 512, "FM", c - 1024) for c in range(3072, 5120, 512)]
            blocks += [(c, 512, "TM", c - 5120 + 1024) for c in range(5120, 6144, 512)]
        uT = kb.sb([128, 16, T], BF16, "uT")
        wb = [kb.sb([128, 16, 512], BF16, "wb") for _ in range(2)]
        pb = [kb.ps([128, 512], F32, "pb") for _ in range(4)]
        stg = [kb.sb([128, 512], BF16, "stg") for _ in range(4)]
        stgf = [kb.sb([128, 16], F32, "stgf") for _ in range(2)]
        stga = [kb.sb([8, 512], F32, "stga") for _ in range(2)]
        cnt = 0
        wi = 0

        def loadw(bi):
            c0, nc_, kind, dst = blocks[bi]
            b = wb[bi % 2]
            kb.dma("gpsimd", b.t[:, :, 0:nc_], W.t[:, c0:c0 + nc_].rearrange("(k p) c -> p k c", p=128),
                   reads=[W], writes=[b], sem_of=b)

        for st in range(S // T):
            for tt in range(T // 128):
                self.norm_transpose(hsrc, st * T + tt * 128, gain, uT, tt * 128, env)
            loadw(0)
            for bi, (c0, nc_, kind, dst) in enumerate(blocks):
                if bi + 1 < len(blocks):
                    loadw(bi + 1)
                b = wb[bi % 2]
                if kind == "FM":
                    for j in range(nc_ // 128):
                        for th in range(T // 512):
                            p = pb[cnt % 4]
                            sg = stg[cnt % 4]
                            cnt += 1
                            for k in range(16):
                                kb.op("tensor", lambda e, k=k, j=j, th=th, p=p, b=b: e.matmul(
                                    p[:], lhsT=b.t[:, k, j * 128:(j + 1) * 128],
                                    rhs=uT.t[:, k, th * 512:(th + 1) * 512], start=(k == 0), stop=(k == 15)),
                                    reads=[b, uT], writes=[p] if k == 0 else (), pwrites=() if k == 0 else [p])
                            self.evac(sg[:], p[:], reads=[p], writes=[sg])
                            r0 = dst + j * 128
                            t0 = st * T + th * 512
                            kb.dma("sync", self.FM.t[r0:r0 + 128, t0:t0 + 512], sg[:], reads=[sg],
                                   pwrites=[self.FM], sem_of=sg)
                elif kind == "TM":
                    for tt in range(T // 128):
                        p = pb[cnt % 4]
                        sg = stg[cnt % 4]
                        cnt += 1
                        for k in range(16):
                            kb.op("tensor", lambda e, k=k, tt=tt, p=p, b=b: e.matmul(
                                p[:], lhsT=uT.t[:, k, tt * 128:(tt + 1) * 128], rhs=b.t[:, k, 0:512],
                                start=(k == 0), stop=(k == 15)),
                                reads=[b, uT], writes=[p] if k == 0 else (), pwrites=() if k == 0 else [p])
                        self.evac(sg[:], p[:], reads=[p], writes=[sg])
                        t0 = st * T + tt * 128
                        kb.dma("sync", self.TM.t[t0:t0 + 128, dst:dst + 512], sg[:], reads=[sg],
                               pwrites=[self.TM], sem_of=sg)
                else:
                    for tt in range(T // 128):
                        p = pb[cnt % 4]
                        sf = stgf[cnt % 2]
                        cnt += 1
                        for k in range(16):
                            kb.op("tensor", lambda e, k=k, tt=tt, p=p, b=b: e.matmul(
                                p[:, 0:16], lhsT=uT.t[:, k, tt * 128:(tt + 1) * 128], rhs=b.t[:, k, 0:16],
                                start=(k == 0), stop=(k == 15)),
                                reads=[b, uT], writes=[p] if k == 0 else (), pwrites=() if k == 0 else [p])
                        self.evac(sf[:], p[:, 0:16], reads=[p], writes=[sf])
                        t0 = st * T + tt * 128
                        kb.dma("sync", self.ABtm.t[t0:t0 + 128, :], sf[:], reads=[sf], pwrites=[self.ABtm],
                               sem_of=sf)
                    for th in range(T // 512):
                        p = pb[cnt % 4]
                        sa = stga[cnt % 2]
                        cnt += 1
                        for k in range(16):
                            kb.op("tensor", lambda e, k=k, th=th, p=p, b=b: e.matmul(
                                p[0:8, :], lhsT=b.t[:, k, 0:8], rhs=uT.t[:, k, th * 512:(th + 1) * 512],
                                start=(k == 0), stop=(k == 15)),
                                reads=[b, uT], writes=[p] if k == 0 else (), pwrites=() if k == 0 else [p])
                        self.evac(sa[:], p[0:8, :], reads=[p], writes=[sa])
                        t0 = st * T + th * 512
                        kb.dma("sync", self.AFM.t[:, t0:t0 + 512], sa[:], reads=[sa], pwrites=[self.AFM],
                               sem_of=sa)
        kb.end_phase()

    def phase_gemm_tm(self, actT, K, W, wrow0, Ydst, TB):
        kb = self.kb
        S = self.S
        KC = K // 128
        kb.begin_phase()
        wb = [kb.sb([128, KC, 512], BF16, "wb") for _ in range(2)]
        ab = [kb.sb([128, KC, TB], BF16, "ab") for _ in range(2)]
        pb = [kb.ps([128, 512], F32, "pb") for _ in range(4)]
        stg = [kb.sb([128, 512], F32, "stg") for _ in range(4)]
        cnt = 0
        ai = 0

        def loadw(cb):
            b = wb[cb % 2]
            kb.dma("gpsimd", b[:], W.t[wrow0:wrow0 + K, cb * 512:(cb + 1) * 512].rearrange("(k p) c -> p k c", p=128),
                   reads=[W], writes=[b], sem_of=b)

        loadw(0)
        for cb in range(4):
            if cb + 1 < 4:
                loadw(cb + 1)
            b = wb[cb % 2]
            for tb in range(S // TB):
                a = ab[ai % 2]
                ai += 1
                kb.dma("sync", a[:], actT.t[0:K, tb * TB:(tb + 1) * TB].rearrange("(k p) t -> p k t", p=128),
                       reads=[actT], writes=[a], sem_of=a)
                for tt in range(TB // 128):
                    p = pb[cnt % 4]
                    sg = stg[cnt % 4]
                    cnt += 1
                    for k in range(KC):
                        kb.op("tensor", lambda e, k=k, tt=tt, p=p, a=a, b=b: e.matmul(
                            p[:], lhsT=a.t[:, k, tt * 128:(tt + 1) * 128], rhs=b.t[:, k, :],
                            start=(k == 0), stop=(k == KC - 1)),
                            reads=[a, b], writes=[p] if k == 0 else (), pwrites=() if k == 0 else [p])
                    self.evac(sg[:], p[:], reads=[p], writes=[sg])
                    t0 = tb * TB + tt * 128
                    kb.dma("sync", Ydst.t[t0:t0 + 128, cb * 512:(cb + 1) * 512], sg[:], reads=[sg],
                           pwrites=[Ydst], sem_of=sg)
        kb.end_phase()

    def phase_norm_res(self, Ysrc, hsrc, which, layer, hdst):
        kb = self.kb
        kb.begin_phase()
        gain = self.load_gain(which, layer)
        mh = kb.sb([128, 1], F32, "mhalf")
        kb.op("gpsimd", lambda e: e.memset(mh[:], -0.5), writes=[mh])
        yt = [kb.sb([128, D], F32, "yt") for _ in range(2)]
        ht = [kb.sb([128, D], F32, "ht") for _ in range(2)]
        tmp = [kb.sb([128, D], F32, "tmp") for _ in range(2)]
        ss = [kb.sb([128, 2], F32, "ss") for _ in range(2)]
        junk = kb.sb([128, D], BF16, "junk")
        for t in range(self.NT):
            y, h, tm, s_ = yt[t % 2], ht[t % 2], tmp[t % 2], ss[t % 2]
            kb.dma("sync", y[:], Ysrc.t[t * 128:(t + 1) * 128, :], reads=[Ysrc], writes=[y], sem_of=y)
            kb.dma("sync", h[:], hsrc.t[t * 128:(t + 1) * 128, :], reads=[hsrc], writes=[h], sem_of=h)
            kb.op("scalar", lambda e: e.activation(out=junk[:], in_=y[:], func=AF.Square, accum_out=s_[:, 0:1]),
                  reads=[y], writes=[junk], pwrites=[s_])
            self.rstd_from_ss(s_, D, mh)
            kb.op("vector", lambda e: e.scalar_tensor_tensor(out=tm[:], in0=y[:], scalar=s_[:, 1:2], in1=gain[:],
                                                             op0=ALU.mult, op1=ALU.mult),
                  reads=[y, s_, gain], writes=[tm])
            kb.op("gpsimd", lambda e: e.tensor_tensor(out=tm[:], in0=tm[:], in1=h[:], op=ALU.add),
                  reads=[tm, h], writes=[tm])
            kb.dma("sync", hdst.t[t * 128:(t + 1) * 128, :], tm[:], reads=[tm], pwrites=[hdst], sem_of=tm)
        kb.end_phase()

    def phase_gateup(self, layer, hsrc):
        kb = self.kb
        S, T = self.S, self.T
        kb.begin_phase()
        env = self.norm_env()
        gain = self.load_gain("ffn_norm_pre", layer)
        Wg, Wu = self.din["ffn_w_gate"], self.din["ffn_w_up"]
        r0w = layer * D
        uT = kb.sb([128, 16, T], BF16, "uT")
        wg = [kb.sb([128, 16, 512], BF16, "wg") for _ in range(2)]
        wu = [kb.sb([128, 16, 512], BF16, "wu") for _ in range(2)]
        pg = [kb.ps([128, 512], F32, "pg") for _ in range(2)]
        pu = [kb.ps([128, 512], F32, "pu") for _ in range(2)]
        sgb = [kb.sb([128, 512], F32, "sg") for _ in range(2)]
        stg = [kb.sb([128, 512], BF16, "stg") for _ in range(4)]
        nfb = FF // 512
        cnt = 0

        def loadw(fb):
            for Wm, bufs in ((Wg, wg), (Wu, wu)):
                b = bufs[fb % 2]
                kb.dma("gpsimd", b[:], Wm.t[r0w:r0w + D, fb * 512:(fb + 1) * 512].rearrange("(k p) c -> p k c", p=128),
                       reads=[Wm], writes=[b], sem_of=b)

        for st in range(S // T):
            for tt in range(T // 128):
                self.norm_transpose(hsrc, st * T + tt * 128, gain, uT, tt * 128, env)
            loadw(0)
            for fb in range(nfb):
                if fb + 1 < nfb:
                    loadw(fb + 1)
                bg, bu = wg[fb % 2], wu[fb % 2]
                for j in range(4):
                    for th in range(T // 512):
                        p1, p2, s1, sg = pg[cnt % 2], pu[cnt % 2], sgb[cnt % 2], stg[cnt % 4]
                        cnt += 1
                        for (p, b) in ((p1, bg), (p2, bu)):
                            for k in range(16):
                                kb.op("tensor", lambda e, k=k, j=j, th=th, p=p, b=b: e.matmul(
                                    p[:], lhsT=b.t[:, k, j * 128:(j + 1) * 128],
                                    rhs=uT.t[:, k, th * 512:(th + 1) * 512], start=(k == 0), stop=(k == 15)),
                                    reads=[b, uT], writes=[p] if k == 0 else (), pwrites=() if k == 0 else [p])
                        kb.op("scalar", lambda e: e.activation(out=s1[:], in_=p1[:], func=AF.Silu),
                              reads=[p1], writes=[s1])
                        kb.op("vector", lambda e: e.tensor_tensor(out=sg[:], in0=s1[:], in1=p2[:], op=ALU.mult),
                              reads=[s1, p2], writes=[sg])
                        rr = (fb * 4 + j) * 128
                        t0 = st * T + th * 512
                        kb.dma("sync", self.AT.t[rr:rr + 128, t0:t0 + 512], sg[:], reads=[sg], pwrites=[self.AT],
                               sem_of=sg)
        kb.end_phase()


def _attn_load_head(P, bufs, hi, qrow, krow, vcol):
    kb = P.kb
    S, NT = P.S, P.NT
    qT, kT, v = bufs[hi % 2]
    kb.dma("sync", qT[:], P.FM.t[qrow:qrow + 128, :], reads=[P.FM], writes=[qT], sem_of=qT)
    kb.dma("sync", kT[:], P.FM.t[krow:krow + 128, :], reads=[P.FM], writes=[kT], sem_of=kT)
    kb.dma("sync", v[:], P.TM.t[:, vcol:vcol + 128].rearrange("(t p) d -> p t d", p=128),
           reads=[P.TM], writes=[v], sem_of=v)
    return qT, kT, v


def _attn_bufs(P):
    kb = P.kb
    S, NT = P.S, P.NT
    return [(kb.sb([128, S], BF16, "qT"), kb.sb([128, S], BF16, "kT"), kb.sb([128, NT, 128], BF16, "v"))
            for _ in range(2)]


def run_pipelined(gens):
    inflight = []
    it = iter(gens)
    done = False
    while not done or inflight:
        if not done:
            try:
                inflight.append(next(it))
            except StopIteration:
                done = True
        nxt = []
        for g in inflight:
            try:
                next(g)
                nxt.append(g)
            except StopIteration:
                pass
        inflight = nxt


def phase_sb(P, nheads=8):
    kb = P.kb
    S = P.S
    kb.begin_phase()
    bufs = _attn_bufs(P)
    cm = P.load_const("cmask", [128, 8 * 512], F32)
    tri = P.load_const("tri", [128, 128], F32)
    ones = P.load_const("ones", [128, 128], F32)
    zb = [kb.ps([128, 512], F32, "z") for _ in range(4)]
    bb = [kb.ps([128, 512], F32, "bt") for _ in range(2)]
    ob = [kb.ps([128, 512], F32, "o") for _ in range(2)]
    NR = 5
    Eb = [kb.sb([128, 512], F32, "E") for _ in range(2)]
    SPb = [kb.sb([128, 512], F32, "SP") for _ in range(NR)]
    tb = [kb.sb([128, 512], F32, "t") for _ in range(NR)]
    Wb = [kb.sb([128, 512], BF16, "W") for _ in range(NR)]
    Cs = [kb.sb([128, 512], F32, "C") for _ in range(3)]
    stg = [kb.sb([128, 512], BF16, "stg") for _ in range(2)]
    heads = list(range(nheads))
    hb = {0: _attn_load_head(P, bufs, 0, heads[0] * 128, 1024 + heads[0] * 128, heads[0] * 128)}
    state = dict(csum=None, ci=0)

    def pair_gen(hi, h, qb, kt, idx, n, c, oc):
        if qb == 1 and idx == 0 and hi + 1 < len(heads):
            h2 = heads[hi + 1]
            hb[hi + 1] = _attn_load_head(P, bufs, hi + 1, h2 * 128, 1024 + h2 * 128, h2 * 128)
        qT, kT, v = hb[hi]
        r = kt - 4 * qb
        Z, B = zb[c % 4], bb[c % 2]
        E, SP, t, Wt = Eb[c % 2], SPb[c % NR], tb[c % NR], Wb[c % NR]
        O, sg = ob[oc % 2], stg[oc % 2]
        kb.op("tensor", lambda e: e.matmul(Z[:], lhsT=kT[:, kt * 128:(kt + 1) * 128],
                                           rhs=qT[:, qb * 512:(qb + 1) * 512], start=True, stop=True),
              reads=[kT, qT], writes=[Z])
        kb.op("scalar", lambda e: e.activation(out=E[:], in_=Z[:], func=AF.Exp, scale=SCALE), reads=[Z], writes=[E])
        kb.op("scalar", lambda e: e.activation(out=SP[:], in_=E[:], func=AF.Ln, bias=1.0, scale=1.0), reads=[E],
              writes=[SP])
        yield
        if idx == 0:
            state["csum"] = None
        csum = state["csum"]
        if r >= 0:
            kb.op("gpsimd", lambda e: e.tensor_tensor(out=SP[:], in0=SP[:], in1=cm[:, r * 512:(r + 1) * 512],
                                                      op=ALU.mult), reads=[SP, cm], writes=[SP])
        kb.op("tensor", lambda e: e.matmul(B[:], lhsT=tri[:], rhs=SP[:], start=True,
                                           stop=(csum is None)), reads=[tri, SP], writes=[B])
        if csum is not None:
            kb.op("tensor", lambda e: e.matmul(B[:], lhsT=ones[:], rhs=csum[:],
                                               start=False, stop=True), reads=[ones, csum], pwrites=[B])
        if idx + 1 < n:
            cn = Cs[state["ci"] % 3]
            state["ci"] += 1
            if csum is None:
                kb.op("gpsimd", lambda e: e.tensor_copy(out=cn[:], in_=SP[:]), reads=[SP], writes=[cn])
            else:
                kb.op("gpsimd", lambda e: e.tensor_tensor(out=cn[:], in0=csum[:], in1=SP[:], op=ALU.add),
                      reads=[csum, SP], writes=[cn])
            state["csum"] = cn
        yield
        kb.op("vector", lambda e: e.scalar_tensor_tensor(out=t[:], in0=Z[:], scalar=SCALE, in1=SP[:],
                                                         op0=ALU.mult, op1=ALU.subtract), reads=[Z, SP], writes=[t])
        kb.op("vector", lambda e: e.tensor_tensor(out=t[:], in0=t[:], in1=B[:], op=ALU.subtract), reads=[t, B],
              writes=[t])
        kb.op("scalar", lambda e: e.activation(out=Wt[:], in_=t[:], func=AF.Exp), reads=[t], writes=[Wt])
        yield
        if r >= 0:
            kb.op("gpsimd", lambda e: e.tensor_tensor(out=Wt[:], in0=Wt[:], in1=cm[:, r * 512:(r + 1) * 512],
                                                      op=ALU.mult), reads=[Wt, cm], writes=[Wt])
        kb.op("tensor", lambda e: e.matmul(O[:], lhsT=v[:, kt, :], rhs=Wt[:], start=(idx == 0), stop=(idx == n - 1)),
              reads=[v, Wt], writes=[O] if idx == 0 else (), pwrites=() if idx == 0 else [O])
        if idx == n - 1:
            P.evac(sg[:], O[:], reads=[O], writes=[sg])
            kb.dma("sync", P.MIX.t[h * 128:(h + 1) * 128, qb * 512:(qb + 1) * 512], sg[:], reads=[sg],
                   pwrites=[P.MIX], sem_of=sg)

    def all_pairs():
        c = 0
        oc = 0
        for hi, h in enumerate(heads):
            for qb in range(S // 512):
                kts = list(range(4 * qb + 3, -1, -1))
                for idx, kt in enumerate(kts):
                    yield pair_gen(hi, h, qb, kt, idx, len(kts), c, oc)
                    c += 1
                oc += 1

    run_pipelined(all_pairs())
    kb.end_phase()


def phase_softmax_attn(P, kind, nheads=8):
    kb = P.kb
    S, NT = P.S, P.NT
    kb.begin_phase()
    bufs = _attn_bufs(P)
    onesb = P.load_const("ones", [128, 128], BF16)
    zb = [kb.ps([128, 512], F32, "z") for _ in range(2)]
    ob = [kb.ps([128, 512], F32, "o") for _ in range(2)]
    db = [kb.ps([128, 512], F32, "dn") for _ in range(2)]
    NR = 3
    Pb = [kb.sb([128, 512], BF16, "P") for _ in range(NR)]
    rec = [kb.sb([128, 512], F32, "rec") for _ in range(2)]
    stg = [kb.sb([128, 512], BF16, "stg") for _ in range(2)]
    if kind == "moba":
        cm = P.load_const("cmaskb", [128, 4 * 512], BF16, src=None, q=None) if False else None
        cmb = kb.sb([128, 4 * 512], BF16, "cmb")
        kb.dma("gpsimd", cmb[:], P.cst["cmask"].t[:, 4 * 512:8 * 512], reads=[P.cst["cmask"]], writes=[cmb],
               sem_of=cmb)
        esel = P.load_const("esel", [128, 16 * 128], BF16)
        gmask = P.load_const("gmask", [128, NT * 16], F32)
        ownm = P.load_const("ownmask", [128, NT * 16], F32)
        identb = P.load_const("ident", [128, 128], BF16)
        gps = kb.ps([128, 512], F32, "gps")
        bps = kb.ps([128, 1024], BF16, "bps")
        kmf = kb.sb([128, 16], F32, "kmf")
        kmb = kb.sb([128, 16], BF16, "kmb")
        g = kb.sb([128, NT * 16], F32, "g")
        m8 = kb.sb([128, NT * 8], F32, "m8")
        sel = kb.sb([128, NT * 16], F32, "sel")
        biasb = kb.sb([128, NT * 128], BF16, "biasb")
        kb.op("gpsimd", lambda e: e.memset(biasb[:], 0.0), writes=[biasb])
        biasT = [kb.sb([128, S], BF16, "biasT") for _ in range(2)]
        qoff, koff, voff, ooff = 0, 1024, 0, 0
        nb = S // 256
    else:
        mfull = P.load_const("mfull", [128, 2944], BF16)
        zb = zb + [kb.ps([128, 512], F32, "z")]
        qoff, koff, voff, ooff = 2048, 3072, 1024, 1024
    heads = list(range(nheads))
    h0 = heads[0]
    hb = {0: _attn_load_head(P, bufs, 0, qoff + h0 * 128, koff + h0 * 128, voff + h0 * 128)}
    NR = 5
    Pb = Pb + [kb.sb([128, 512], BF16, "P"), kb.sb([128, 512], BF16, "P")]

    def prologue(hi):
        qT, kT, v = hb[hi]
        bT = biasT[hi % 2]
        kb.op("vector", lambda e: e.tensor_reduce(out=kmf[:, 0:nb], in_=kT.t[:, :].rearrange("p (n k) -> p n k", k=256),
                                                  axis=AX.X, op=ALU.add), reads=[kT], writes=[kmf])
        if nb < 16:
            kb.op("vector", lambda e: e.memset(kmf[:, nb:16], 0.0), pwrites=[kmf])
        kb.op("vector", lambda e: e.tensor_scalar(out=kmb[:], in0=kmf[:], scalar1=1.0 / 256, scalar2=None,
                                                  op0=ALU.mult), reads=[kmf], writes=[kmb])
        for t in range(NT):
            kb.op("tensor", lambda e, t=t: e.matmul(gps[:, t * 16:(t + 1) * 16], lhsT=qT[:, t * 128:(t + 1) * 128],
                                                     rhs=kmb[:], start=True, stop=True),
                  reads=[qT, kmb], writes=[gps] if t == 0 else (), pwrites=() if t == 0 else [gps])
        kb.op("vector", lambda e: e.tensor_tensor(out=g[:], in0=gps[:, 0:NT * 16], in1=gmask[:], op=ALU.add),
              reads=[gps, gmask], writes=[g])
        for t in range(NT):
            kb.op("vector", lambda e, t=t: e.max(out=m8[:, t * 8:(t + 1) * 8], in_=g[:, t * 16:(t + 1) * 16]),
                  reads=[g], pwrites=[m8])
        for t in range(NT):
            kb.op("vector", lambda e, t=t: e.tensor_scalar(out=sel[:, t * 16:(t + 1) * 16], in0=g[:, t * 16:(t + 1) * 16],
                                                            scalar1=m8[:, t * 8 + 2:t * 8 + 3], scalar2=None,
                                                            op0=ALU.is_ge), reads=[g, m8], pwrites=[sel])
        kb.op("vector", lambda e: e.tensor_tensor(out=sel[:], in0=sel[:], in1=ownm[:], op=ALU.max),
              reads=[sel, ownm], writes=[sel])
        kb.op("vector", lambda e: e.tensor_scalar(
            out=biasb.t[:, :].rearrange("p (t c) -> p t c", c=128)[:, :, 0:16],
            in0=sel.t[:, :].rearrange("p (t c) -> p t c", c=16), scalar1=-1.0, scalar2=-NEG,
            op0=ALU.add, op1=ALU.mult), reads=[sel], writes=[biasb])
        for t0 in range(0, NT, 8):
            nt8 = min(8, NT - t0)
            for t in range(t0, t0 + nt8):
                kb.op("tensor", lambda e, t=t: e.transpose(bps[:, (t - t0) * 128:(t - t0 + 1) * 128],
                                                            biasb[:, t * 128:(t + 1) * 128], identb[:]),
                      reads=[biasb, identb], writes=[bps] if t == t0 else (), pwrites=() if t == t0 else [bps])
            P.evac(bT[:, t0 * 128:(t0 + nt8) * 128], bps[:, 0:nt8 * 128], reads=[bps], pwrites=[bT])

    pairs = []
    c = 0
    oc = 0
    for hi, h in enumerate(heads):
        for qb in range(S // 512):
            if kind == "moba":
                kts = list(range(0, 4 * qb + 4))
            else:
                kts = list(range(max(0, 4 * qb - 16), 4 * qb + 4))
            for idx, kt in enumerate(kts):
                pairs.append(dict(hi=hi, h=h, qb=qb, kt=kt, idx=idx, n=len(kts), c=c, oc=oc,
                                  first_of_head=(qb == 0 and idx == 0), second_of_head=(qb == 0 and idx == 1)))
                c += 1
            oc += 1
    NZ = len(zb)

    def pair_gen(p):
        hi, h, qb, kt, idx, c = p["hi"], p["h"], p["qb"], p["kt"], p["idx"], p["c"]
        if qb == 1 and idx == 0 and hi + 1 < len(heads):
            h2 = heads[hi + 1]
            hb[hi + 1] = _attn_load_head(P, bufs, hi + 1, qoff + h2 * 128, koff + h2 * 128, voff + h2 * 128)
        if p["first_of_head"] and kind == "moba":
            prologue(hi)
        qT, kT, v = hb[hi]
        r = kt - 4 * qb
        Z = zb[c % NZ]
        Pt = Pb[c % NR]
        O, Dn = ob[p["oc"] % 2], db[p["oc"] % 2]
        sg, rc = stg[p["oc"] % 2], rec[p["oc"] % 2]
        first, last = idx == 0, idx == p["n"] - 1
        if kind == "moba":
            bT = biasT[hi % 2]
            n = kt // 2
            kb.op("tensor", lambda e: e.matmul(Z[:], lhsT=kT[:, kt * 128:(kt + 1) * 128],
                                               rhs=qT[:, qb * 512:(qb + 1) * 512], start=True, stop=False),
                  reads=[kT, qT], writes=[Z])
            kb.op("tensor", lambda e: e.matmul(Z[:], lhsT=esel[:, n * 128:(n + 1) * 128],
                                               rhs=bT[:, qb * 512:(qb + 1) * 512], start=False, stop=True),
                  reads=[esel, bT], pwrites=[Z])
        else:
            kb.op("tensor", lambda e: e.matmul(Z[:], lhsT=kT[:, kt * 128:(kt + 1) * 128],
                                               rhs=qT[:, qb * 512:(qb + 1) * 512], start=True, stop=True),
                  reads=[kT, qT], writes=[Z])
        yield
        kb.op("scalar", lambda e: e.activation(out=Pt[:], in_=Z[:], func=AF.Exp, scale=SCALE), reads=[Z], writes=[Pt])
        if kind == "moba":
            if r >= 0:
                kb.op("vector", lambda e: e.tensor_tensor(out=Pt[:], in0=Pt[:], in1=cmb[:, r * 512:(r + 1) * 512],
                                                          op=ALU.mult), reads=[Pt, cmb], writes=[Pt])
        else:
            x0 = (qb * 512 - kt * 128) + 384
            eng = "vector" if c % 2 == 0 else "gpsimd"
            kb.op(eng, lambda e: e.tensor_tensor(out=Pt[:], in0=Pt[:], in1=mfull[:, x0:x0 + 512], op=ALU.mult),
                  reads=[Pt, mfull], writes=[Pt])
        yield
        kb.op("tensor", lambda e: e.matmul(O[:], lhsT=v[:, kt, :], rhs=Pt[:], start=first, stop=last),
              reads=[v, Pt], writes=[O] if first else (), pwrites=() if first else [O])
        kb.op("tensor", lambda e: e.matmul(Dn[:], lhsT=onesb[:], rhs=Pt[:], start=first, stop=last),
              reads=[onesb, Pt], writes=[Dn] if first else (), pwrites=() if first else [Dn])
        if last:
            kb.op("vector", lambda e: e.reciprocal(out=rc[:], in_=Dn[:]), reads=[Dn], writes=[rc])
            kb.op("vector", lambda e: e.tensor_tensor(out=sg[:], in0=rc[:], in1=O[:], op=ALU.mult), reads=[rc, O],
                  writes=[sg])
            r0 = ooff + h * 128
            kb.dma("sync", P.MIX.t[r0:r0 + 128, qb * 512:(qb + 1) * 512], sg[:], reads=[sg], pwrites=[P.MIX],
                   sem_of=sg)

    run_pipelined(pair_gen(p) for p in pairs)
    kb.end_phase()


def phase_gdn(P, nheads=8):
    import os
    STOP = int(os.environ.get("GDN_STOP", "99"))
    H5S = int(os.environ.get("H5_STOP", "99"))
    kb = P.kb
    S, NT = P.S, P.NT
    NC = S // 64
    kb.begin_phase()
    pz = [kb.ps([128, 512], F32, "pz") for _ in range(6)]
    pzb = [kb.ps([128, 1024], BF16, "pzb") for _ in range(2)]
    identf = P.load_const("ident", [128, 128], F32)
    identb = P.load_const("ident", [128, 128], BF16)
    onesb = P.load_const("ones", [128, 128], BF16)
    sel8 = P.load_const("sel8", [128, 8 * 128], F32)
    negst = P.load_const("negstrict", [128, 128], F32)
    neginc = P.load_const("negincT", [128, 128], F32)
    rowm = P.load_const("rowmask", [128, 2], F32)
    mh = kb.sb([128, 1], F32, "mhalf")
    kb.op("gpsimd", lambda e: e.memset(mh[:], -0.5), writes=[mh])

    gam = kb.sb([128, S], F32, "gam")
    gamT = kb.sb([128, NT * 8], F32, "gamT")
    glT = kb.sb([128, NT * 8], F32, "glT")
    eg = kb.sb([128, NT * 8], F32, "eg")
    kdf = kb.sb([128, NT * 8], F32, "kdf")
    beta = kb.sb([128, NT * 8], F32, "beta")
    nbeta = kb.sb([128, NT * 8], F32, "nbeta")
    beg = kb.sb([128, NT * 8], F32, "beg")
    egm = kb.sb([128, 2, NT * 8], F32, "egm")
    decS = kb.sb([128, 8, NC], F32, "decS")
    cw = kb.sb([128, 24, 4], F32, "cw")
    onw = kb.sb([128, 128], F32, "onw")
    kb.push_scope()
    scanm = P.load_const("scanmask", [128, S], F32)
    tmpA = kb.sb([128, S], F32, "tmpA")
    tmpB = kb.sb([128, S], F32, "tmpB")
    kb.op("vector", lambda e: e.memset(tmpA[:], 0.0), writes=[tmpA])
    kb.dma("sync", tmpA[0:8, :], P.AFM[:, :], reads=[P.AFM], writes=[tmpA], sem_of=tmpA)
    sc8 = kb.sb([128, 4], F32, "sc8")
    kb.op("vector", lambda e: e.memset(sc8[:], 0.0), writes=[sc8])
    kb.dma("sync", sc8[0:8, 0:1], P.din["ev_a_log"].t.rearrange("o h -> h o"), reads=[P.din["ev_a_log"]],
           writes=[sc8], sem_of=sc8)
    kb.dma("sync", sc8[0:8, 1:2], P.din["ev_dt_bias"].t.rearrange("o h -> h o"), reads=[P.din["ev_dt_bias"]],
           writes=[sc8], sem_of=sc8)
    kb.op("scalar", lambda e: e.activation(out=sc8[:, 2:3], in_=sc8[:, 0:1], func=AF.Exp), reads=[sc8], pwrites=[sc8])
    kb.op("vector", lambda e: e.tensor_scalar(out=sc8[:, 3:4], in0=sc8[:, 2:3], scalar1=-1.0, scalar2=None,
                                              op0=ALU.mult), reads=[sc8], pwrites=[sc8])
    kb.op("scalar", lambda e: e.activation(out=tmpB[:], in_=tmpA[:], func=AF.Exp, bias=sc8[:, 1:2], scale=1.0),
          reads=[tmpA, sc8], writes=[tmpB])
    kb.op("scalar", lambda e: e.activation(out=tmpB[:], in_=tmpB[:], func=AF.Ln, bias=1.0, scale=1.0),
          reads=[tmpB], writes=[tmpB])
    kb.op("vector", lambda e: e.tensor_scalar(out=tmpB[:], in0=tmpB[:], scalar1=sc8[:, 3:4], scalar2=None,
                                              op0=ALU.mult), reads=[tmpB, sc8], writes=[tmpB])
    kb.op("vector", lambda e: e.tensor_tensor_scan(out=gam[:], data0=scanm[:], data1=tmpB[:], initial=0.0,
                                                   op0=ALU.mult, op1=ALU.add), reads=[scanm, tmpB], writes=[gam])
    gv = gam.t[:, :].rearrange("p (n c) -> p n c", c=64)
    kb.op("vector", lambda e: e.tensor_copy(out=tmpA.t[:, :].rearrange("p (n c) -> p n c", c=64),
                                            in_=gv[:, :, 63:64].to_broadcast([128, NC, 64])),
          reads=[gam], writes=[tmpA])
    for (src, dst) in ((gam, gamT), (tmpA, glT)):
        for t0 in range(0, NT, 4):
            pp = pz[(t0 // 4) % 2]
            for t in range(t0, t0 + 4):
                kb.op("tensor", lambda e, t=t: e.transpose(pp[:, (t - t0) * 128:(t - t0 + 1) * 128],
                                                            src[:, t * 128:(t + 1) * 128], identf[:]),
                      reads=[src, identf], writes=[pp] if t == t0 else (), pwrites=() if t == t0 else [pp])
            P.evac(dst.t[:, t0 * 8:(t0 + 4) * 8].rearrange("p (t h) -> p t h", h=8),
                   pp.t[:, :].rearrange("p (t c) -> p t c", c=128)[:, :, 0:8], reads=[pp], pwrites=[dst])
    kb.op("scalar", lambda e: e.activation(out=eg[:], in_=gamT[:], func=AF.Exp), reads=[gamT], writes=[eg])
    kb.op("vector", lambda e: e.tensor_tensor(out=kdf[:], in0=glT[:], in1=gamT[:], op=ALU.subtract),
          reads=[glT, gamT], writes=[kdf])
    kb.op("scalar", lambda e: e.activation(out=kdf[:], in_=kdf[:], func=AF.Exp), reads=[kdf], writes=[kdf])
    ab = kb.sb([128, NT, 16], F32, "ab")
    kb.dma("sync", ab[:], P.ABtm.t[:, :].rearrange("(t p) c -> p t c", p=128), reads=[P.ABtm], writes=[ab], sem_of=ab)
    kb.op("scalar", lambda e: e.activation(out=beta.t[:, :].rearrange("p (t h) -> p t h", h=8), in_=ab[:, :, 8:16],
                                           func=AF.Sigmoid), reads=[ab], writes=[beta])
    kb.op("vector", lambda e: e.tensor_scalar(out=nbeta[:], in0=beta[:], scalar1=-1.0, scalar2=None, op0=ALU.mult),
          reads=[beta], writes=[nbeta])
    kb.op("vector", lambda e: e.tensor_tensor(out=beg[:], in0=beta[:], in1=eg[:], op=ALU.mult),
          reads=[beta, eg], writes=[beg])
    for hf in range(2):
        kb.op("vector", lambda e, hf=hf: e.tensor_scalar(out=egm[:, hf, :], in0=eg[:], scalar1=rowm[:, hf:hf + 1],
                                                          scalar2=None, op0=ALU.mult), reads=[eg, rowm], pwrites=[egm])
    glc = kb.sb([128, NC], F32, "glc")
    kb.op("vector", lambda e: e.tensor_copy(out=glc[:], in_=gv[:, :, 63]), reads=[gam], writes=[glc])
    for h in range(8):
        pp = pz[h % 2]
        kb.op("tensor", lambda e: e.matmul(pp[:, 0:NC], lhsT=sel8[:, h * 128:(h + 1) * 128], rhs=glc[:], start=True,
                                           stop=True), reads=[sel8, glc], writes=[pp])
        kb.op("scalar", lambda e: e.activation(out=decS[:, h, :], in_=pp[:, 0:NC], func=AF.Exp), reads=[pp],
              pwrites=[decS])
    cw4 = kb.sb([128, 3072], F32, "cw4")
    kb.op("gpsimd", lambda e: e.memset(cw4[:], 0.0), writes=[cw4])
    kb.dma("sync", cw4[0:4, :], P.din["ev_conv_w"][:, :], reads=[P.din["ev_conv_w"]], writes=[cw4], sem_of=cw4)
    for t0 in range(0, 24, 4):
        pp = pz[(t0 // 4) % 2]
        for t in range(t0, t0 + 4):
            kb.op("tensor", lambda e, t=t: e.transpose(pp[:, (t - t0) * 128:(t - t0 + 1) * 128],
                                                        cw4[:, t * 128:(t + 1) * 128], identf[:]),
                  reads=[cw4, identf], writes=[pp] if t == t0 else (), pwrites=() if t == t0 else [pp])
        P.evac(cw[:, t0:t0 + 4, :], pp.t[:, :].rearrange("p (t c) -> p t c", c=128)[:, :, 0:4], reads=[pp], pwrites=[cw])
    kb.dma("sync", onw[:], P.din["ev_onorm"].t[0:1, :].to_broadcast([128, 128]), reads=[P.din["ev_onorm"]],
           writes=[onw], sem_of=onw)

    kb.pop_scope()
    if STOP == 0:
        kb.end_phase()
        return
    xin = [kb.sb([128, S], BF16, "xin") for _ in range(2)]
    acc = kb.sb([128, S], F32, "acc")
    qnT = kb.sb([128, S], BF16, "qnT")
    knT = kb.sb([128, S], BF16, "knT")
    vcT = kb.sb([128, S], BF16, "vcT")
    kn_tm = kb.sb([128, NT, 128], BF16, "kn_tm")
    v_tm = kb.sb([128, NT, 128], BF16, "v_tm")
    z_tm = kb.sb([128, NT, 128], BF16, "z_tm")
    kd = kb.sb([128, NT, 128], BF16, "kd")
    ITb = kb.sb([128, NT, 128], BF16, "IT")
    ub = kb.sb([128, NT, 128], F32, "u")
    wT0 = kb.sb([128, NT, 128], BF16, "wT0")
    wT1 = kb.sb([128, NT, 128], BF16, "wT1")
    kb.op("gpsimd", lambda e: e.memset(wT0[:], 0.0), writes=[wT0])
    kb.op("gpsimd", lambda e: e.memset(wT1[:], 0.0), writes=[wT1])
    oacc = kb.sb([128, NT, 128], F32, "oacc")
    sq = [kb.sb([128, 512], BF16, "sq") for _ in range(2)]
    rr = [kb.sb([128, 512], F32, "rr") for _ in range(2)]
    LANES = 3
    lanes = []
    for L in range(LANES):
        ln = dict(kg=kb.sb([128, 128], BF16, "kbg"), vb=kb.sb([128, 128], BF16, "vb"),
                  a1=kb.sb([128, 128], F32, "e1"), a2=kb.sb([128, 128], F32, "e2"),
                  N=[kb.sb([128, 128], F32, "N") for _ in range(2)],
                  NT=[kb.sb([128, 128], F32, "NTm") for _ in range(2)],
                  P=[kb.sb([128, 128], F32, "Pm") for _ in range(2)],
                  MT=kb.sb([128, 128], BF16, "MT"))
        ln["bankA"] = pz[2 * L]
        ln["bankB"] = pz[2 * L + 1]
        lanes.append(ln)
    Sf = kb.sb([128, 128], F32, "Sf")
    Sb = kb.sb([128, 128], BF16, "Sb")
    vnew = [kb.sb([128, 128], BF16, "vnew") for _ in range(2)]
    ot = [kb.sb([128, 128], F32, "ot") for _ in range(2)]
    ot2 = [kb.sb([128, 128], F32, "ot2") for _ in range(2)]
    ss = [kb.sb([128, 2], F32, "ss") for _ in range(2)]
    junk = kb.sb([128, 128], BF16, "junk")
    yo = [kb.sb([128, 128], F32, "yo") for _ in range(2)]
    zs = [kb.sb([128, 128], F32, "zs") for _ in range(2)]
    yb = [kb.sb([128, 128], BF16, "ybo") for _ in range(2)]
    stg = [kb.sb([128, 512], BF16, "stg") for _ in range(2)]
    LNQ = float(np.log(HD ** -0.5))
    xi = 0
    for h in range(nheads):
        kb.dma("sync", z_tm[:], P.TM.t[:, 1024 + h * 128:1024 + (h + 1) * 128].rearrange("(t p) d -> p t d", p=128),
               reads=[P.TM], writes=[z_tm], sem_of=z_tm)
        for gi, dstT in enumerate((qnT, knT, vcT)):
            x = xin[xi % 2]
            xi += 1
            ct = gi * 8 + h
            r0 = 2048 + ct * 128
            kb.dma("sync", x[:], P.FM.t[r0:r0 + 128, :], reads=[P.FM], writes=[x], sem_of=x)
            kb.op("vector", lambda e: e.tensor_scalar(out=acc[:], in0=x[:], scalar1=cw[:, ct, 3:4], scalar2=None,
                                                      op0=ALU.mult), reads=[x, cw], writes=[acc])
            for sft in (1, 2, 3):
                kb.op("vector", lambda e, sft=sft: e.scalar_tensor_tensor(
                    out=acc[:, sft:S], in0=x[:, 0:S - sft], scalar=cw[:, ct, 3 - sft:4 - sft], in1=acc[:, sft:S],
                    op0=ALU.mult, op1=ALU.add), reads=[x, cw, acc], writes=[acc])
            kb.op("scalar", lambda e: e.activation(out=dstT[:], in_=acc[:], func=AF.Silu), reads=[acc], writes=[dstT])
        if STOP == 1:
            continue
        bi = 0
        for (xT, lnb) in ((qnT, LNQ), (knT, 0.0)):
            for blk in range(S // 512):
                s_, r_, pp = sq[bi % 2], rr[bi % 2], pz[bi % 2]
                bi += 1
                sl = slice(blk * 512, (blk + 1) * 512)
                kb.op("gpsimd", lambda e: e.tensor_tensor(out=s_[:], in0=xT[:, sl], in1=xT[:, sl], op=ALU.mult),
                      reads=[xT], writes=[s_])
                kb.op("tensor", lambda e: e.matmul(pp[:], lhsT=onesb[:], rhs=s_[:], start=True, stop=True),
                      reads=[onesb, s_], writes=[pp])
                kb.op("scalar", lambda e: e.activation(out=r_[:], in_=pp[:], func=AF.Ln, bias=EPS, scale=1.0),
                      reads=[pp], writes=[r_])
                kb.op("scalar", lambda e: e.activation(out=r_[:], in_=r_[:], func=AF.Exp, bias=lnb, scale=-0.5),
                      reads=[r_], writes=[r_])
                kb.op("vector", lambda e: e.tensor_tensor(out=xT[:, sl], in0=xT[:, sl], in1=r_[:], op=ALU.mult),
                      reads=[xT, r_], writes=[xT])
        if STOP == 2:
            continue
        for (srcT, dst) in ((knT, kn_tm), (vcT, v_tm)):
            for t0 in range(0, NT, 8):
                pp = pzb[(t0 // 8) % 2]
                n8 = min(8, NT - t0)
                for t in range(t0, t0 + n8):
                    kb.op("tensor", lambda e, t=t: e.transpose(pp[:, (t - t0) * 128:(t - t0 + 1) * 128],
                                                                srcT[:, t * 128:(t + 1) * 128], identb[:]),
                          reads=[srcT, identb], writes=[pp] if t == t0 else (), pwrites=() if t == t0 else [pp])
                P.evac(dst.t[:, t0:t0 + n8, :], pp.t[:, 0:n8 * 128].rearrange("p (t c) -> p t c", c=128), reads=[pp],
                       pwrites=[dst])
        if STOP == 3:
            continue
        kb.barrier()

        def tile_gen(i, L, h=h):
            col = i * 8 + h
            tl = slice(i * 128, (i + 1) * 128)
            ln = lanes[L]
            kg, vb_, a1, a2, MT = ln["kg"], ln["vb"], ln["a1"], ln["a2"], ln["MT"]
            bA, bB = ln["bankA"], ln["bankB"]
            A0, A1 = bA.t[:, 0:128], bA.t[:, 128:256]
            B0, B1, B2 = bB.t[:, 0:128], bB.t[:, 128:256], bB.t[:, 256:384]
            kb.op("gpsimd", lambda e: e.tensor_scalar(out=kg[:], in0=kn_tm[:, i, :], scalar1=beg[:, col:col + 1],
                                                      scalar2=None, op0=ALU.mult), reads=[kn_tm, beg], writes=[kg])
            kb.op("gpsimd", lambda e: e.tensor_scalar(out=vb_[:], in0=v_tm[:, i, :], scalar1=beta[:, col:col + 1],
                                                      scalar2=None, op0=ALU.mult), reads=[v_tm, beta], writes=[vb_])
            kb.op("gpsimd", lambda e: e.tensor_scalar(out=kd[:, i, :], in0=kn_tm[:, i, :], scalar1=kdf[:, col:col + 1],
                                                      scalar2=None, op0=ALU.mult), reads=[kn_tm, kdf], pwrites=[kd])
            kb.op("tensor", lambda e: e.matmul(B0, lhsT=knT[:, tl], rhs=knT[:, tl], start=True, stop=True),
                  reads=[knT], writes=[bB])
            kb.op("tensor", lambda e: e.matmul(B1, lhsT=knT[:, tl], rhs=qnT[:, tl], start=True, stop=True),
                  reads=[knT, qnT], writes=[bB])
            kb.op("tensor", lambda e: e.matmul(B2, lhsT=sel8[:, h * 128:(h + 1) * 128], rhs=gam[:, tl],
                                               start=True, stop=True), reads=[sel8, gam], writes=[bB])
            yield
            kb.op("vector", lambda e: e.tensor_scalar(out=a1[:], in0=B2, scalar1=gamT[:, col:col + 1],
                                                      scalar2=None, op0=ALU.subtract), reads=[bB, gamT], writes=[a1])
            kb.op("vector", lambda e: e.tensor_scalar(out=a2[:], in0=B2, scalar1=gamT[:, col:col + 1],
                                                      scalar2=None, op0=ALU.subtract), reads=[bB, gamT], writes=[a2])
            yield
            kb.op("vector", lambda e: e.scalar_tensor_tensor(out=a1[:], in0=a1[:], scalar=0.0, in1=negst[:],
                                                             op0=ALU.max, op1=ALU.subtract), reads=[a1, negst],
                  writes=[a1])
            kb.op("vector", lambda e: e.scalar_tensor_tensor(out=a2[:], in0=a2[:], scalar=0.0, in1=neginc[:],
                                                             op0=ALU.min, op1=ALU.add), reads=[a2, neginc], writes=[a2])
            yield
            kb.op("scalar", lambda e: e.activation(out=a1[:], in_=a1[:], func=AF.Exp, scale=-1.0), reads=[a1],
                  writes=[a1])
            kb.op("scalar", lambda e: e.activation(out=a2[:], in_=a2[:], func=AF.Exp), reads=[a2], writes=[a2])
            yield
            NTk, Nk = ln["NT"][0], ln["N"][0]
            kb.op("vector", lambda e: e.scalar_tensor_tensor(out=NTk[:], in0=B0, scalar=nbeta[:, col:col + 1],
                                                             in1=a1[:], op0=ALU.mult, op1=ALU.mult),
                  reads=[bB, nbeta, a1], writes=[NTk])
            kb.op("vector", lambda e: e.tensor_tensor(out=ITb[:, i, :], in0=a2[:], in1=B1, op=ALU.mult),
                  reads=[a2, bB], pwrites=[ITb])
            yield
            kb.op("tensor", lambda e: e.transpose(A1, NTk[:], identf[:]), reads=[NTk, identf], writes=[bA])
            yield
            kb.op("scalar", lambda e: e.activation(out=Nk[:], in_=A1, func=AF.Copy), reads=[bA], writes=[Nk])
            yield
            Pm = ln["P"][0]
            kb.op("gpsimd", lambda e: e.tensor_tensor(out=Pm[:], in0=identf[:], in1=Nk[:], op=ALU.add),
                  reads=[identf, Nk], writes=[Pm])
            for lvl in range(5):
                last = lvl == 4
                NT2, N2 = ln["NT"][(lvl + 1) % 2], ln["N"][(lvl + 1) % 2]
                kb.op("tensor", lambda e: e.matmul(A0, lhsT=Nk[:], rhs=NTk[:], start=True, stop=True),
                      reads=[Nk, NTk], writes=[bA])
                if not last:
                    kb.op("tensor", lambda e: e.matmul(B0, lhsT=NTk[:], rhs=Nk[:], start=True, stop=True),
                          reads=[Nk, NTk], writes=[bB])
                yield
                kb.op("scalar", lambda e: e.activation(out=NT2[:], in_=A0, func=AF.Copy), reads=[bA], writes=[NT2])
                if not last:
                    kb.op("vector", lambda e: e.tensor_copy(out=N2[:], in_=B0), reads=[bB], writes=[N2])
                yield
                kb.op("tensor", lambda e: e.matmul(B2, lhsT=NT2[:], rhs=Pm[:], start=True, stop=True),
                      reads=[NT2, Pm], writes=[bB])
                yield
                if last:
                    kb.op("vector", lambda e: e.tensor_tensor(out=MT[:], in0=Pm[:], in1=B2, op=ALU.add),
                          reads=[Pm, bB], writes=[MT])
                else:
                    Pn = ln["P"][(lvl + 1) % 2]
                    kb.op("vector", lambda e: e.tensor_tensor(out=Pn[:], in0=Pm[:], in1=B2, op=ALU.add),
                          reads=[Pm, bB], writes=[Pn])
                    Pm = Pn
                NTk, Nk = NT2, N2
            yield
            kb.op("tensor", lambda e: e.matmul(A0, lhsT=MT[:], rhs=vb_[:], start=True, stop=True),
                  reads=[MT, vb_], writes=[bA])
            kb.op("tensor", lambda e: e.matmul(B1, lhsT=kg[:], rhs=MT[:], start=True, stop=True),
                  reads=[MT, kg], writes=[bB])
            yield
            kb.op("scalar", lambda e: e.activation(out=ub[:, i, :], in_=A0, func=AF.Copy), reads=[bA], pwrites=[ub])
            kb.op("vector", lambda e: e.tensor_copy(out=wT0[:, i, 0:64], in_=B1[:, 0:64]), reads=[bB], pwrites=[wT0])
            kb.op("vector", lambda e: e.tensor_copy(out=wT1[:, i, 64:128], in_=B1[:, 64:128]), reads=[bB],
                  pwrites=[wT1])

        free_lanes = list(range(LANES))
        active = []
        ti = 0
        while ti < NT or active:
            while ti < NT and free_lanes:
                L = free_lanes.pop(0)
                active.append((tile_gen(ti, L), L))
                ti += 1
            nxt = []
            for g, L in active:
                try:
                    next(g)
                    nxt.append((g, L))
                except StopIteration:
                    free_lanes.append(L)
            active = nxt
        kb.barrier()
        if STOP == 4:
            continue
        kb.op("vector", lambda e: e.memset(Sf[:], 0.0), writes=[Sf])
        kb.op("vector", lambda e: e.memset(Sb[:], 0.0), writes=[Sb])
        for n in range(NC):
            i, hf = n // 2, n % 2
            col = i * 8 + h
            tl = slice(i * 128, (i + 1) * 128)
            wTp = wT0 if hf == 0 else wT1
            pWS, pO1, pO2, pSU = pz[0 + 4 * 0], pz[1], pz[2], pz[3]
            vn, o1, o2 = vnew[n % 2], ot[n % 2], ot2[n % 2]
            kb.op("tensor", lambda e: e.matmul(pWS[:, 0:128], lhsT=wTp[:, i, :], rhs=Sb[:], start=True, stop=True),
                  reads=[wTp, Sb], writes=[pWS])
            kb.op("tensor", lambda e: e.matmul(pO1[:, 0:128], lhsT=qnT[:, tl], rhs=Sb[:], start=True, stop=True),
                  reads=[qnT, Sb], writes=[pO1])
            kb.op("vector", lambda e: e.scalar_tensor_tensor(out=vn[:], in0=ub[:, i, :], scalar=rowm[:, hf:hf + 1],
                                                             in1=pWS[:, 0:128], op0=ALU.mult, op1=ALU.subtract),
                  reads=[ub, rowm, pWS], writes=[vn])
            kb.op("tensor", lambda e: e.matmul(pSU[:, 0:128], lhsT=kd[:, i, :], rhs=vn[:], start=True, stop=True),
                  reads=[kd, vn], writes=[pSU])
            kb.op("tensor", lambda e: e.matmul(pO2[:, 0:128], lhsT=ITb[:, i, :], rhs=vn[:], start=True, stop=True),
                  reads=[ITb, vn], writes=[pO2])
            kb.op("vector", lambda e: e.scalar_tensor_tensor(out=Sf[:], in0=Sf[:], scalar=decS[:, h, n:n + 1],
                                                             in1=pSU[:, 0:128], op0=ALU.mult, op1=ALU.add),
                  reads=[Sf, decS, pSU], writes=[Sf])
            kb.op("scalar", lambda e: e.activation(out=Sb[:], in_=Sf[:], func=AF.Copy), reads=[Sf], writes=[Sb])
            kb.op("vector", lambda e: e.tensor_scalar(out=o1[:], in0=pO1[:, 0:128], scalar1=egm[:, hf, col:col + 1],
                                                      scalar2=None, op0=ALU.mult), reads=[pO1, egm], writes=[o1])
            if hf == 0:
                kb.op("vector", lambda e: e.tensor_tensor(out=oacc[:, i, :], in0=o1[:], in1=pO2[:, 0:128], op=ALU.add),
                      reads=[o1, pO2], pwrites=[oacc])
            else:
                kb.op("vector", lambda e: e.tensor_tensor(out=o2[:], in0=o1[:], in1=pO2[:, 0:128], op=ALU.add),
                      reads=[o1, pO2], writes=[o2])
                kb.op("gpsimd", lambda e: e.tensor_tensor(out=oacc[:, i, :], in0=oacc[:, i, :], in1=o2[:], op=ALU.add),
                      reads=[oacc, o2], pwrites=[oacc])
        if STOP == 5:
            continue
        for i in range(NT):
            s_, y_, z_, yb_ = ss[i % 2], yo[i % 2], zs[i % 2], yb[i % 2]
            kb.op("scalar", lambda e: e.activation(out=junk[:], in_=oacc[:, i, :], func=AF.Square, accum_out=s_[:, 0:1]),
                  reads=[oacc], writes=[junk], pwrites=[s_])
            P.rstd_from_ss(s_, HD, mh)
            kb.op("vector", lambda e: e.scalar_tensor_tensor(out=y_[:], in0=oacc[:, i, :], scalar=s_[:, 1:2], in1=onw[:],
                                                             op0=ALU.mult, op1=ALU.mult), reads=[oacc, s_, onw],
                  writes=[y_])
            kb.op("scalar", lambda e: e.activation(out=z_[:], in_=z_tm[:, i, :], func=AF.Silu), reads=[z_tm], writes=[z_])
            kb.op("vector", lambda e: e.tensor_tensor(out=yb_[:], in0=y_[:], in1=z_[:], op=ALU.mult), reads=[y_, z_],
                  writes=[yb_])
            pp = pzb[(i // 4) % 2]
            j = i % 4
            kb.op("tensor", lambda e: e.transpose(pp[:, j * 128:(j + 1) * 128], yb_[:], identb[:]), reads=[yb_, identb],
                  writes=[pp] if j == 0 else (), pwrites=() if j == 0 else [pp])
            if j == 3:
                sg = stg[(i // 4) % 2]
                P.evac(sg[:], pp[:, 0:512], reads=[pp], writes=[sg])
                r0 = 1024 + h * 128
                t0 = (i // 4) * 512
                kb.dma("sync", P.MIX.t[r0:r0 + 128, t0:t0 + 512], sg[:], reads=[sg], pwrites=[P.MIX], sem_of=sg)
        kb.barrier()
    kb.end_phase()


def build_full(S):
    P = Prog(S)
    x = P.din["x"]
    P.phase_inproj(0, x)
    phase_sb(P)
    phase_gdn(P)
    P.phase_gemm_tm(P.MIX, D, P.din["ev_w_out"], 0, P.Y, 512)
    P.phase_norm_res(P.Y, x, "mix_norm_post", 0, P.H1)
    P.phase_gateup(0, P.H1)
    P.phase_gemm_tm(P.AT, FF, P.din["ffn_w_down"], 0, P.Y, 512)
    P.phase_norm_res(P.Y, P.H1, "ffn_norm_post", 0, P.H2)
    P.phase_inproj(1, P.H2)
    phase_softmax_attn(P, "moba")
    phase_softmax_attn(P, "dil")
    P.phase_gemm_tm(P.MIX, D, P.din["od_w_out"], 0, P.Y, 512)
    P.phase_norm_res(P.Y, P.H2, "mix_norm_post", 1, P.H3)
    P.phase_gateup(1, P.H3)
    P.phase_gemm_tm(P.AT, FF, P.din["ffn_w_down"], FF, P.Y, 512)
    P.phase_norm_res(P.Y, P.H3, "ffn_norm_post", 1, P.OUT)
    return P


def make_in_map(inputs, b, S, consts):
    m = {"x": np.ascontiguousarray(np.asarray(inputs["x"])[b, :S], dtype=np.float32)}
    for n, shp in WEIGHT_SPECS:
        m[n] = np.ascontiguousarray(np.asarray(inputs[n], dtype=np.float32).reshape(shp))
    for n, v in consts.items():
        m["c_" + n] = v
    return m


def kernel(**inputs):
    x = np.asarray(inputs["x"])
    B, S, _ = x.shape
    P = build_full(S)
    consts = make_consts(S)
    in_maps = [make_in_map(inputs, b, S, consts) for b in range(B)]
    res = run_bass_kernel_spmd(P.nc, in_maps, core_ids=list(range(B)))
    out = np.stack([np.asarray(res.results[b]["y"], dtype=np.float32) for b in range(B)], axis=0)
    return out
```

```python
from contextlib import ExitStack
import numpy as np
import concourse.bass as bass
import concourse.mybir as mybir
from concourse.bass_utils import run_bass_kernel_spmd

F32 = mybir.dt.float32
BF16 = mybir.dt.bfloat16
F32R = mybir.dt.float32r
AF = mybir.ActivationFunctionType
ALU = mybir.AluOpType
AX = mybir.AxisListType

D = 2048
FF = 5632
HD = 128
EPS = 1e-6
EVEN_IN = 7184
ODD_IN = 6144
SCALE = HD ** -0.5
NEG = -30000.0


class Buf:
    __slots__ = ("t", "w", "r", "name", "dsem")

    def __init__(self, t, name):
        self.t = t
        self.w = {}
        self.r = {}
        self.name = name
        self.dsem = None

    def __getitem__(self, idx):
        return self.t[idx]


class KB:
    def __init__(self, nc):
        self.nc = nc
        self.st = {}
        self.sems = {}
        for n in ["tensor", "vector", "scalar", "gpsimd", "sync"]:
            h = nc.alloc_semaphore("s_" + n)
            self.sems[h.num] = [h, 0, False]
            self.st[n] = dict(e=getattr(nc, n), sem=h, seen={})
        self.nbuf = 0
        self.nwait = 0
        self.nop = 0
        self.dsem_pool = {"sw": [], "hw": []}
        self.es = None

    def _nm(self, p):
        self.nbuf += 1
        return "%s%d" % (p, self.nbuf)

    def sb(self, shape, dt, name=None):
        name = self._nm(name or "b")
        t = self.es.enter_context(self.nc.sbuf_tensor(name, list(shape), dt))
        return Buf(t, name)

    def ps(self, shape, dt=F32, name=None):
        name = self._nm(name or "p")
        t = self.es.enter_context(self.nc.psum_tensor(name, list(shape), dt))
        return Buf(t, name)

    def dram(self, name, shape, dt, kind="Internal"):
        t = self.nc.dram_tensor(name, list(shape), dt, kind=kind)
        return Buf(t.ap(), name)

    def _waits(self, eng, reads, writes, pwrites):
        st = self.st[eng]
        own = st["sem"].num
        skip_own = eng == "tensor"
        need = {}
        for b in reads:
            for s, v in b.w.items():
                if need.get(s, 0) < v:
                    need[s] = v
        for b in writes:
            for s, v in b.w.items():
                if need.get(s, 0) < v:
                    need[s] = v
            for s, v in b.r.items():
                if need.get(s, 0) < v:
                    need[s] = v
        for b in pwrites:
            for s, v in b.r.items():
                if need.get(s, 0) < v:
                    need[s] = v
        if skip_own:
            need.pop(own, None)
        seen = st["seen"]
        e = st["e"]
        for s, v in need.items():
            info = self.sems[s]
            if info[2]:
                v = info[1]
            if seen.get(s, 0) >= v:
                continue
            e.wait_ge(info[0], v)
            seen[s] = v
            self.nwait += 1

    def op(self, eng, fn, reads=(), writes=(), pwrites=()):
        self._waits(eng, reads, writes, pwrites)
        st = self.st[eng]
        ins = fn(st["e"])
        h = st["sem"]
        info = self.sems[h.num]
        info[1] += 1
        ins.then_inc(h, 1)
        c = info[1]
        for b in reads:
            b.r[h.num] = c
        for b in writes:
            b.w[h.num] = c
        for b in pwrites:
            b.w[h.num] = c
        self.nop += 1
        return ins

    def dma(self, q, out_ap, in_ap, reads=(), writes=(), pwrites=(), sem_of=None):
        self._waits(q, reads, writes, pwrites)
        b0 = sem_of
        kind = "sw" if q == "gpsimd" else "hw"
        if b0.dsem is None:
            if self.dsem_pool[kind]:
                h = self.dsem_pool[kind].pop()
            else:
                h = self.nc.alloc_semaphore(self._nm("d"))
                self.sems[h.num] = [h, 0, True]
            b0.dsem = h
            b0_kind = kind
            self.phase_dsems.append((kind, h))
        h = b0.dsem
        info = self.sems[h.num]
        info[1] += 16
        ins = self.st[q]["e"].dma_start(out=out_ap, in_=in_ap)
        ins.then_inc(h, 16)
        c = info[1]
        for b in reads:
            b.r[h.num] = c
        for b in writes:
            b.w[h.num] = c
        for b in pwrites:
            b.w[h.num] = c
        self.nop += 1
        return ins

    def barrier(self):
        for n, st in self.st.items():
            own = st["sem"].num
            for s, info in self.sems.items():
                if s == own or info[1] == 0:
                    continue
                if st["seen"].get(s, 0) >= info[1]:
                    continue
                st["e"].wait_ge(info[0], info[1])
                st["seen"][s] = info[1]
                self.nwait += 1

    def push_scope(self):
        self.es_stack = getattr(self, "es_stack", [])
        self.es_stack.append(self.es)
        self.es = ExitStack()

    def pop_scope(self):
        self.barrier()
        self.es.close()
        self.es = self.es_stack.pop()

    def begin_phase(self):
        self.es = ExitStack()
        self.phase_dsems = []

    def end_phase(self):
        self.barrier()
        self.es.close()
        self.es = None
        for kind, h in self.phase_dsems:
            self.dsem_pool[kind].append(h)
        self.phase_dsems = []


def make_consts(S):
    NT = S // 128
    c = {}
    c["ident"] = np.eye(128, dtype=np.float32)
    kp = np.arange(128)[:, None]
    qf = np.arange(512)[None, :]
    cm = np.zeros((128, 8, 512), np.float32)
    for r in range(4):
        cm[:, r, :] = (r * 128 + kp) < qf
        cm[:, 4 + r, :] = (r * 128 + kp) <= qf
    c["cmask"] = cm.reshape(128, 8 * 512)
    s = np.arange(128)[:, None]
    j = np.arange(128)[None, :]
    c["tri"] = (s > j).astype(np.float32)
    c["ones"] = np.ones((128, 128), np.float32)
    en = np.zeros((128, 16, 128), np.float32)
    for n in range(16):
        en[n, n, :] = 1.0
    c["esel"] = en.reshape(128, 16 * 128)
    gm = np.zeros((128, NT, 16), np.float32)
    om = np.zeros((128, NT, 16), np.float32)
    for t in range(NT):
        own = t // 2
        gm[:, t, own:] = -1e30
        if own < 16:
            om[:, t, own] = 1.0
    c["gmask"] = gm.reshape(128, NT * 16)
    c["ownmask"] = om.reshape(128, NT * 16)
    x = np.arange(2944)[None, :]
    dlt = x - 384 - kp
    m = ((dlt >= 0) & (dlt <= 128)).astype(np.float32)
    m += ((dlt >= 0) & (dlt <= 512) & (dlt % 4 == 0))
    m += ((dlt >= 0) & (dlt <= 2048) & (dlt % 16 == 0))
    c["mfull"] = m.astype(np.float32)
    sel8 = np.zeros((128, 8, 128), np.float32)
    for h in range(8):
        sel8[h, h, :] = 1.0
    c["sel8"] = sel8.reshape(128, 8 * 128)
    rm = np.ones((128, S), np.float32)
    rm[:, ::64] = 0.0
    c["scanmask"] = rm
    blk = (s // 64) == (j // 64)
    c["negstrict"] = np.where(blk & (s > j), 0.0, NEG).astype(np.float32)
    c["negincT"] = np.where(blk & (j >= s), 0.0, NEG).astype(np.float32)
    rowm = np.zeros((128, 2), np.float32)
    rowm[:64, 0] = 1.0
    rowm[64:, 1] = 1.0
    c["rowmask"] = rowm
    return c


CONST_SHAPES = lambda S: {k: v.shape for k, v in make_consts(S).items()}

WEIGHT_SPECS = [
    ("mix_norm_pre", (2, D)), ("mix_norm_post", (2, D)), ("ffn_norm_pre", (2, D)), ("ffn_norm_post", (2, D)),
    ("ev_w_in", (D, EVEN_IN)), ("ev_conv_w", (4, 3072)), ("ev_a_log", (1, 8)), ("ev_dt_bias", (1, 8)),
    ("ev_onorm", (1, 128)), ("ev_w_out", (D, D)), ("od_w_in", (D, ODD_IN)), ("od_w_out", (D, D)),
    ("ffn_w_gate", (2 * D, FF)), ("ffn_w_up", (2 * D, FF)), ("ffn_w_down", (2 * FF, D)),
]


class Prog:
    def __init__(self, S, io=None, declare_weights=True):
        self.S = S
        self.NT = S // 128
        self.T = min(1024, S)
        io = io or {}
        self.nc = bass.Bass("TRN2", target_bir_lowering=False)
        self.kb = KB(self.nc)
        kb = self.kb
        self.din = {}
        self.din["x"] = kb.dram("x", [S, D], F32, kind="ExternalInput")
        for n, shp in WEIGHT_SPECS:
            if declare_weights or shp[0] * shp[1] < 100000:
                self.din[n] = kb.dram(n, list(shp), F32, kind="ExternalInput")
        self.cst = {}
        for n, shp in CONST_SHAPES(S).items():
            self.cst[n] = kb.dram("c_" + n, list(shp), F32, kind="ExternalInput")

        def scratch(name, shape, dt):
            kind = io.get(name, "Internal")
            return kb.dram(name, shape, dt, kind=kind)

        self.FM = scratch("FM", [5120, S], BF16)
        self.TM = scratch("TM", [S, 2048], BF16)
        self.ABtm = scratch("ABtm", [S, 16], F32)
        self.AFM = scratch("AFM", [8, S], F32)
        self.MIX = scratch("MIX", [2048, S], BF16)
        self.Y = scratch("Y", [S, D], F32)
        self.AT = scratch("AT", [FF, S], BF16)
        self.H1 = scratch("H1", [S, D], F32)
        self.H2 = scratch("H2", [S, D], F32)
        self.H3 = scratch("H3", [S, D], F32)
        self.OUT = kb.dram("y", [S, D], F32, kind="ExternalOutput")
        self.evc = 0

    def evac(self, out_ap, in_ap, reads, writes=(), pwrites=(), scale=None):
        kb = self.kb
        self.evc += 1
        if self.evc % 2 == 0:
            if scale is None:
                kb.op("scalar", lambda e: e.activation(out=out_ap, in_=in_ap, func=AF.Copy),
                      reads=reads, writes=writes, pwrites=pwrites)
            else:
                kb.op("scalar", lambda e: e.activation(out=out_ap, in_=in_ap, func=AF.Copy, scale=scale),
                      reads=reads, writes=writes, pwrites=pwrites)
        else:
            if scale is None:
                kb.op("vector", lambda e: e.tensor_copy(out=out_ap, in_=in_ap),
                      reads=reads, writes=writes, pwrites=pwrites)
            else:
                kb.op("vector", lambda e: e.tensor_scalar(out=out_ap, in0=in_ap, scalar1=scale, scalar2=None,
                                                          op0=ALU.mult),
                      reads=reads, writes=writes, pwrites=pwrites)

    def load_const(self, name, shape, dt, src=None, q=None):
        kb = self.kb
        b = kb.sb(shape, dt, "c_" + name)
        src = src if src is not None else self.cst[name]
        if q is None:
            q = "gpsimd" if dt != F32 else "sync"
        kb.dma(q, b[:], src[:], reads=[src], writes=[b], sem_of=b)
        return b

    def load_gain(self, which, layer):
        kb = self.kb
        src = self.din[which]
        g = kb.sb([128, D], F32, "gain")
        kb.dma("sync", g[:], src.t[layer:layer + 1, :].to_broadcast([128, D]), reads=[src], writes=[g], sem_of=g)
        return g

    def rstd_from_ss(self, ss, n, mhalf, extra_scale=None):
        kb = self.kb
        kb.op("gpsimd", lambda e: e.tensor_scalar(out=ss[:, 1:2], in0=ss[:, 0:1], scalar1=1.0 / n, scalar2=EPS,
                                                  op0=ALU.mult, op1=ALU.add), reads=[ss], pwrites=[ss])
        kb.op("gpsimd", lambda e: e.tensor_tensor(out=ss[:, 1:2], in0=ss[:, 1:2], in1=mhalf[:, 0:1], op=ALU.pow),
              reads=[ss, mhalf], pwrites=[ss])

    def norm_transpose(self, hsrc, tok0, gain, uT, col0, env):
        kb = self.kb
        i = env["i"]
        env["i"] += 1
        xt = env["xt"][i % 2]
        ss = env["ss"][i % 2]
        yb = env["yb"][i % 2]
        pT = env["pT"]
        kb.dma("sync", xt[:], hsrc.t[tok0:tok0 + 128, :], reads=[hsrc], writes=[xt], sem_of=xt)
        kb.op("scalar", lambda e: e.activation(out=env["junk"][:], in_=xt[:], func=AF.Square,
                                               accum_out=ss[:, 0:1]),
              reads=[xt], writes=[env["junk"]], pwrites=[ss])
        self.rstd_from_ss(ss, D, env["mhalf"])
        kb.op("vector", lambda e: e.scalar_tensor_tensor(out=yb[:], in0=xt[:], scalar=ss[:, 1:2], in1=gain[:],
                                                         op0=ALU.mult, op1=ALU.mult),
              reads=[xt, ss, gain], writes=[yb])
        ident = env["identb"]
        for k in range(16):
            kb.op("tensor", lambda e, k=k: e.transpose(pT[:, k * 128:(k + 1) * 128], yb[:, k * 128:(k + 1) * 128],
                                                        ident[:]),
                  reads=[yb, ident], pwrites=[pT])
        for hf in range(2):
            self.evac(uT.t[:, hf * 8:(hf + 1) * 8, col0:col0 + 128],
                      pT.t[:, hf * 1024:(hf + 1) * 1024].rearrange("p (k c) -> p k c", c=128),
                      reads=[pT], pwrites=[uT])

    def norm_env(self):
        kb = self.kb
        env = dict(i=0)
        env["xt"] = [kb.sb([128, D], F32, "xt") for _ in range(2)]
        env["ss"] = [kb.sb([128, 2], F32, "ss") for _ in range(2)]
        env["yb"] = [kb.sb([128, D], BF16, "yb") for _ in range(2)]
        env["junk"] = kb.sb([128, D], BF16, "junk")
        env["pT"] = kb.ps([128, D], BF16, "pT")
        env["identb"] = self.load_const("ident", [128, 128], BF16)
        mh = kb.sb([128, 1], F32, "mhalf")
        kb.op("gpsimd", lambda e: e.memset(mh[:], -0.5), writes=[mh])
        env["mhalf"] = mh
        return env

    def phase_inproj(self, layer, hsrc):
        kb = self.kb
        S, T = self.S, self.T
        kb.begin_phase()
        env = self.norm_env()
        gain = self.load_gain("mix_norm_pre", layer)
        W = self.din["ev_w_in"] if layer == 0 else self.din["od_w_in"]
        if layer == 0:
            blocks = [(c, 512, "FM", c) for c in range(0, 2048, 512)]
            blocks += [(c, 512, "TM", c - 2048) for c in range(2048, 3072, 512)]
            blocks += [(c, 512, "FM", c - 1024) for c in range(3072, 6144, 512)]
            blocks += [(c, 512, "TM", c - 6144 + 1024) for c in range(6144, 7168, 512)]
            blocks += [(7168, 16, "AB", 0)]
        else:
            blocks = [(c, 512, "FM", c) for c in range(0, 2048, 512)]
            blocks += [(c, 512, "TM", c - 2048) for c in range(2048, 3072, 512)]
            blocks += [(c, 512, "FM", c - 1024) for c in range(3072, 5120, 512)]
            blocks += [(c, 512, "TM", c - 5120 + 1024) for c in range(5120, 6144, 512)]
        uT = kb.sb([128, 16, T], BF16, "uT")
        wb = [kb.sb([128, 16, 512], BF16, "wb") for _ in range(2)]
        pb = [kb.ps([128, 512], F32, "pb") for _ in range(4)]
        stg = [kb.sb([128, 512], BF16, "stg") for _ in range(4)]
        stgf = [kb.sb([128, 16], F32, "stgf") for _ in range(2)]
        stga = [kb.sb([8, 512], F32, "stga") for _ in range(2)]
        cnt = 0
        wi = 0

        def loadw(bi):
            c0, nc_, kind, dst = blocks[bi]
            b = wb[bi % 2]
            kb.dma("gpsimd", b.t[:, :, 0:nc_], W.t[:, c0:c0 + nc_].rearrange("(k p) c -> p k c", p=128),
                   reads=[W], writes=[b], sem_of=b)

        for st in range(S // T):
            for tt in range(T // 128):
                self.norm_transpose(hsrc, st * T + tt * 128, gain, uT, tt * 128, env)
            loadw(0)
            for bi, (c0, nc_, kind, dst) in enumerate(blocks):
                if bi + 1 < len(blocks):
                    loadw(bi + 1)
                b = wb[bi % 2]
                if kind == "FM":
                    for j in range(nc_ // 128):
                        for th in range(T // 512):
                            p = pb[cnt % 4]
                            sg = stg[cnt % 4]
                            cnt += 1
                            for k in range(16):
                                kb.op("tensor", lambda e, k=k, j=j, th=th, p=p, b=b: e.matmul(
                                    p[:], lhsT=b.t[:, k, j * 128:(j + 1) * 128],
                                    rhs=uT.t[:, k, th * 512:(th + 1) * 512], start=(k == 0), stop=(k == 15)),
                                    reads=[b, uT], writes=[p] if k == 0 else (), pwrites=() if k == 0 else [p])
                            self.evac(sg[:], p[:], reads=[p], writes=[sg])
                            r0 = dst + j * 128
                            t0 = st * T + th * 512
                            kb.dma("sync", self.FM.t[r0:r0 + 128, t0:t0 + 512], sg[:], reads=[sg],
                                   pwrites=[self.FM], sem_of=sg)
                elif kind == "TM":
                    for tt in range(T // 128):
                        p = pb[cnt % 4]
                        sg = stg[cnt % 4]
                        cnt += 1
                        for k in range(16):
                            kb.op("tensor", lambda e, k=k, tt=tt, p=p, b=b: e.matmul(
                                p[:], lhsT=uT.t[:, k, tt * 128:(tt + 1) * 128], rhs=b.t[:, k, 0:512],
                                start=(k == 0), stop=(k == 15)),
                                reads=[b, uT], writes=[p] if k == 0 else (), pwrites=() if k == 0 else [p])
                        self.evac(sg[:], p[:], reads=[p], writes=[sg])
                        t0 = st * T + tt * 128
                        kb.dma("sync", self.TM.t[t0:t0 + 128, dst:dst + 512], sg[:], reads=[sg],
                               pwrites=[self.TM], sem_of=sg)
                else:
                    for tt in range(T // 128):
                        p = pb[cnt % 4]
                        sf = stgf[cnt % 2]
                        cnt += 1
                        for k in range(16):
                            kb.op("tensor", lambda e, k=k, tt=tt, p=p, b=b: e.matmul(
                                p[:, 0:16], lhsT=uT.t[:, k, tt * 128:(tt + 1) * 128], rhs=b.t[:, k, 0:16],
                                start=(k == 0), stop=(k == 15)),
                                reads=[b, uT], writes=[p] if k == 0 else (), pwrites=() if k == 0 else [p])
                        self.evac(sf[:], p[:, 0:16], reads=[p], writes=[sf])
                        t0 = st * T + tt * 128
                        kb.dma("sync", self.ABtm.t[t0:t0 + 128, :], sf[:], reads=[sf], pwrites=[self.ABtm],
                               sem_of=sf)
                    for th in range(T // 512):
                        p = pb[cnt % 4]
                        sa = stga[cnt % 2]
                        cnt += 1
                        for k in range(16):
                            kb.op("tensor", lambda e, k=k, th=th, p=p, b=b: e.matmul(
                                p[0:8, :], lhsT=b.t[:, k, 0:8], rhs=uT.t[:, k, th * 512:(th + 1) * 512],
                                start=(k == 0), stop=(k == 15)),
                                reads=[b, uT], writes=[p] if k == 0 else (), pwrites=() if k == 0 else [p])
                        self.evac(sa[:], p[0:8, :], reads=[p], writes=[sa])
                        t0 = st * T + th * 512
                        kb.dma("sync", self.AFM.t[:, t0:t0 + 512], sa[:], reads=[sa], pwrites=[self.AFM],
                               sem_of=sa)
        kb.end_phase()

    def phase_gemm_tm(self, actT, K, W, wrow0, Ydst, TB):
        kb = self.kb
        S = self.S
        KC = K // 128
        kb.begin_phase()
        wb = [kb.sb([128, KC, 512], BF16, "wb") for _ in range(2)]
        ab = [kb.sb([128, KC, TB], BF16, "ab") for _ in range(2)]
        pb = [kb.ps([128, 512], F32, "pb") for _ in range(4)]
        stg = [kb.sb([128, 512], F32, "stg") for _ in range(4)]
        cnt = 0
        ai = 0

        def loadw(cb):
            b = wb[cb % 2]
            kb.dma("gpsimd", b[:], W.t[wrow0:wrow0 + K, cb * 512:(cb + 1) * 512].rearrange("(k p) c -> p k c", p=128),
                   reads=[W], writes=[b], sem_of=b)

        loadw(0)
        for cb in range(4):
            if cb + 1 < 4:
                loadw(cb + 1)
            b = wb[cb % 2]
            for tb in range(S // TB):
                a = ab[ai % 2]
                ai += 1
                kb.dma("sync", a[:], actT.t[0:K, tb * TB:(tb + 1) * TB].rearrange("(k p) t -> p k t", p=128),
                       reads=[actT], writes=[a], sem_of=a)
                for tt in range(TB // 128):
                    p = pb[cnt % 4]
                    sg = stg[cnt % 4]
                    cnt += 1
                    for k in range(KC):
                        kb.op("tensor", lambda e, k=k, tt=tt, p=p, a=a, b=b: e.matmul(
                            p[:], lhsT=a.t[:, k, tt * 128:(tt + 1) * 128], rhs=b.t[:, k, :],
                            start=(k == 0), stop=(k == KC - 1)),
                            reads=[a, b], writes=[p] if k == 0 else (), pwrites=() if k == 0 else [p])
                    self.evac(sg[:], p[:], reads=[p], writes=[sg])
                    t0 = tb * TB + tt * 128
                    kb.dma("sync", Ydst.t[t0:t0 + 128, cb * 512:(cb + 1) * 512], sg[:], reads=[sg],
                           pwrites=[Ydst], sem_of=sg)
        kb.end_phase()

    def phase_outproj_fused(self, layer, W, hsrc, hdst):
        kb = self.kb
        S = self.S
        kb.begin_phase()
        gain = self.load_gain("mix_norm_post", layer)
        mh = kb.sb([128, 1], F32, "mhalf")
        kb.op("gpsimd", lambda e: e.memset(mh[:], -0.5), writes=[mh])
        wres = [kb.sb([128, 16, 512], BF16, "wres") for _ in range(4)]
        for cb in range(4):
            kb.dma("gpsimd", wres[cb][:], W.t[:, cb * 512:(cb + 1) * 512].rearrange("(k p) c -> p k c", p=128),
                   reads=[W], writes=[wres[cb]], sem_of=wres[cb])
        ab = [kb.sb([128, 16, 512], BF16, "ab") for _ in range(2)]
        pp = [[kb.ps([128, 512], F32, "pp") for _ in range(4)] for _ in range(2)]
        ht = [kb.sb([128, D], F32, "ht") for _ in range(2)]
        tmp = [kb.sb([128, D], F32, "tmp") for _ in range(2)]
        ss = [kb.sb([128, 2], F32, "ss") for _ in range(2)]
        s4 = [kb.sb([128, 4], F32, "s4") for _ in range(2)]
        junk = kb.sb([128, 512], BF16, "junk")
        for tb in range(S // 512):
            a = ab[tb % 2]
            kb.dma("sync", a[:], self.MIX.t[:, tb * 512:(tb + 1) * 512].rearrange("(k p) t -> p k t", p=128),
                   reads=[self.MIX], writes=[a], sem_of=a)
            for tt in range(4):
                t = tb * 4 + tt
                ps4, h, tm, s_, q4 = pp[t % 2], ht[t % 2], tmp[t % 2], ss[t % 2], s4[t % 2]
                kb.dma("sync", h[:], hsrc.t[t * 128:(t + 1) * 128, :], reads=[hsrc], writes=[h], sem_of=h)
                for cb in range(4):
                    p = ps4[cb]
                    for k in range(16):
                        kb.op("tensor", lambda e, k=k, cb=cb, p=p: e.matmul(
                            p[:], lhsT=a.t[:, k, tt * 128:(tt + 1) * 128], rhs=wres[cb].t[:, k, :],
                            start=(k == 0), stop=(k == 15)),
                            reads=[a, wres[cb]], writes=[p] if k == 0 else (), pwrites=() if k == 0 else [p])
                    kb.op("scalar", lambda e, cb=cb, p=p: e.activation(out=junk[:], in_=p[:], func=AF.Square,
                                                                       accum_out=q4[:, cb:cb + 1]),
                          reads=[p], writes=[junk], pwrites=[q4])
                kb.op("vector", lambda e: e.tensor_reduce(out=s_[:, 0:1], in_=q4[:], axis=AX.X, op=ALU.add),
                      reads=[q4], pwrites=[s_])
                self.rstd_from_ss(s_, D, mh)
                for cb in range(4):
                    p = ps4[cb]
                    kb.op("vector", lambda e, cb=cb, p=p: e.scalar_tensor_tensor(
                        out=tm[:, cb * 512:(cb + 1) * 512], in0=p[:], scalar=s_[:, 1:2],
                        in1=gain[:, cb * 512:(cb + 1) * 512], op0=ALU.mult, op1=ALU.mult),
                        reads=[p, s_, gain], pwrites=[tm])
                kb.op("gpsimd", lambda e: e.tensor_tensor(out=tm[:], in0=tm[:], in1=h[:], op=ALU.add),
                      reads=[tm, h], writes=[tm])
                kb.dma("sync", hdst.t[t * 128:(t + 1) * 128, :], tm[:], reads=[tm], pwrites=[hdst], sem_of=tm)
        kb.end_phase()

    def phase_norm_res(self, Ysrc, hsrc, which, layer, hdst):
        kb = self.kb
        kb.begin_phase()
        gain = self.load_gain(which, layer)
        mh = kb.sb([128, 1], F32, "mhalf")
        kb.op("gpsimd", lambda e: e.memset(mh[:], -0.5), writes=[mh])
        yt = [kb.sb([128, D], F32, "yt") for _ in range(2)]
        ht = [kb.sb([128, D], F32, "ht") for _ in range(2)]
        tmp = [kb.sb([128, D], F32, "tmp") for _ in range(2)]
        ss = [kb.sb([128, 2], F32, "ss") for _ in range(2)]
        junk = kb.sb([128, D], BF16, "junk")
        for t in range(self.NT):
            y, h, tm, s_ = yt[t % 2], ht[t % 2], tmp[t % 2], ss[t % 2]
            kb.dma("sync", y[:], Ysrc.t[t * 128:(t + 1) * 128, :], reads=[Ysrc], writes=[y], sem_of=y)
            kb.dma("sync", h[:], hsrc.t[t * 128:(t + 1) * 128, :], reads=[hsrc], writes=[h], sem_of=h)
            kb.op("scalar", lambda e: e.activation(out=junk[:], in_=y[:], func=AF.Square, accum_out=s_[:, 0:1]),
                  reads=[y], writes=[junk], pwrites=[s_])
            self.rstd_from_ss(s_, D, mh)
            kb.op("vector", lambda e: e.scalar_tensor_tensor(out=tm[:], in0=y[:], scalar=s_[:, 1:2], in1=gain[:],
                                                             op0=ALU.mult, op1=ALU.mult),
                  reads=[y, s_, gain], writes=[tm])
            kb.op("gpsimd", lambda e: e.tensor_tensor(out=tm[:], in0=tm[:], in1=h[:], op=ALU.add),
                  reads=[tm, h], writes=[tm])
            kb.dma("sync", hdst.t[t * 128:(t + 1) * 128, :], tm[:], reads=[tm], pwrites=[hdst], sem_of=tm)
        kb.end_phase()

    def phase_gateup(self, layer, hsrc):
        kb = self.kb
        S, T = self.S, self.T
        kb.begin_phase()
        env = self.norm_env()
        gain = self.load_gain("ffn_norm_pre", layer)
        Wg, Wu = self.din["ffn_w_gate"], self.din["ffn_w_up"]
        r0w = layer * D
        uT = kb.sb([128, 16, T], BF16, "uT")
        wg = [kb.sb([128, 16, 512], BF16, "wg") for _ in range(2)]
        wu = [kb.sb([128, 16, 512], BF16, "wu") for _ in range(2)]
        pg = [kb.ps([128, 512], F32, "pg") for _ in range(2)]
        pu = [kb.ps([128, 512], F32, "pu") for _ in range(2)]
        sgb = [kb.sb([128, 512], F32, "sg") for _ in range(2)]
        stg = [kb.sb([128, 512], BF16, "stg") for _ in range(4)]
        nfb = FF // 512
        cnt = 0

        def loadw(fb):
            for Wm, bufs in ((Wg, wg), (Wu, wu)):
                b = bufs[fb % 2]
                kb.dma("gpsimd", b[:], Wm.t[r0w:r0w + D, fb * 512:(fb + 1) * 512].rearrange("(k p) c -> p k c", p=128),
                       reads=[Wm], writes=[b], sem_of=b)

        for st in range(S // T):
            for tt in range(T // 128):
                self.norm_transpose(hsrc, st * T + tt * 128, gain, uT, tt * 128, env)
            loadw(0)
            for fb in range(nfb):
                if fb + 1 < nfb:
                    loadw(fb + 1)
                bg, bu = wg[fb % 2], wu[fb % 2]
                for j in range(4):
                    for th in range(T // 512):
                        p1, p2, s1, sg = pg[cnt % 2], pu[cnt % 2], sgb[cnt % 2], stg[cnt % 4]
                        cnt += 1
                        for (p, b) in ((p1, bg), (p2, bu)):
                            for k in range(16):
                                kb.op("tensor", lambda e, k=k, j=j, th=th, p=p, b=b: e.matmul(
                                    p[:], lhsT=b.t[:, k, j * 128:(j + 1) * 128],
                                    rhs=uT.t[:, k, th * 512:(th + 1) * 512], start=(k == 0), stop=(k == 15)),
                                    reads=[b, uT], writes=[p] if k == 0 else (), pwrites=() if k == 0 else [p])
                        kb.op("scalar", lambda e: e.activation(out=s1[:], in_=p1[:], func=AF.Silu),
                              reads=[p1], writes=[s1])
                        kb.op("vector", lambda e: e.tensor_tensor(out=sg[:], in0=s1[:], in1=p2[:], op=ALU.mult),
                              reads=[s1, p2], writes=[sg])
                        rr = (fb * 4 + j) * 128
                        t0 = st * T + th * 512
                        kb.dma("sync", self.AT.t[rr:rr + 128, t0:t0 + 512], sg[:], reads=[sg], pwrites=[self.AT],
                               sem_of=sg)
        kb.end_phase()


def _attn_load_head(P, bufs, hi, qrow, krow, vcol):
    kb = P.kb
    S, NT = P.S, P.NT
    qT, kT, v = bufs[hi % 2]
    kb.dma("sync", qT[:], P.FM.t[qrow:qrow + 128, :], reads=[P.FM], writes=[qT], sem_of=qT)
    kb.dma("sync", kT[:], P.FM.t[krow:krow + 128, :], reads=[P.FM], writes=[kT], sem_of=kT)
    kb.dma("sync", v[:], P.TM.t[:, vcol:vcol + 128].rearrange("(t p) d -> p t d", p=128),
           reads=[P.TM], writes=[v], sem_of=v)
    return qT, kT, v


def _attn_bufs(P):
    kb = P.kb
    S, NT = P.S, P.NT
    return [(kb.sb([128, S], BF16, "qT"), kb.sb([128, S], BF16, "kT"), kb.sb([128, NT, 128], BF16, "v"))
            for _ in range(2)]


def run_pipelined(gens):
    inflight = []
    it = iter(gens)
    done = False
    while not done or inflight:
        if not done:
            try:
                inflight.append(next(it))
            except StopIteration:
                done = True
        nxt = []
        for g in inflight:
            try:
                next(g)
                nxt.append(g)
            except StopIteration:
                pass
        inflight = nxt


def phase_sb(P, nheads=8):
    kb = P.kb
    S = P.S
    kb.begin_phase()
    bufs = _attn_bufs(P)
    cm = P.load_const("cmask", [128, 8 * 512], F32)
    tri = P.load_const("tri", [128, 128], F32)
    ones = P.load_const("ones", [128, 128], F32)
    zb = [kb.ps([128, 512], F32, "z") for _ in range(4)]
    bb = [kb.ps([128, 512], F32, "bt") for _ in range(2)]
    ob = [kb.ps([128, 512], F32, "o") for _ in range(2)]
    NR = 5
    Eb = [kb.sb([128, 512], F32, "E") for _ in range(2)]
    SPb = [kb.sb([128, 512], F32, "SP") for _ in range(NR)]
    tb = [kb.sb([128, 512], F32, "t") for _ in range(NR)]
    Wb = [kb.sb([128, 512], BF16, "W") for _ in range(NR)]
    Cs = [kb.sb([128, 512], F32, "C") for _ in range(3)]
    stg = [kb.sb([128, 512], BF16, "stg") for _ in range(2)]
    heads = list(range(nheads))
    hb = {0: _attn_load_head(P, bufs, 0, heads[0] * 128, 1024 + heads[0] * 128, heads[0] * 128)}
    state = dict(csum=None, ci=0)

    def pair_gen(hi, h, qb, kt, idx, n, c, oc):
        if qb == 1 and idx == 0 and hi + 1 < len(heads):
            h2 = heads[hi + 1]
            hb[hi + 1] = _attn_load_head(P, bufs, hi + 1, h2 * 128, 1024 + h2 * 128, h2 * 128)
        qT, kT, v = hb[hi]
        r = kt - 4 * qb
        Z, B = zb[c % 4], bb[c % 2]
        E, SP, t, Wt = Eb[c % 2], SPb[c % NR], tb[c % NR], Wb[c % NR]
        O, sg = ob[oc % 2], stg[oc % 2]
        kb.op("tensor", lambda e: e.matmul(Z[:], lhsT=kT[:, kt * 128:(kt + 1) * 128],
                                           rhs=qT[:, qb * 512:(qb + 1) * 512], start=True, stop=True),
              reads=[kT, qT], writes=[Z])
        kb.op("scalar", lambda e: e.activation(out=E[:], in_=Z[:], func=AF.Exp, scale=SCALE), reads=[Z], writes=[E])
        kb.op("scalar", lambda e: e.activation(out=SP[:], in_=E[:], func=AF.Ln, bias=1.0, scale=1.0), reads=[E],
              writes=[SP])
        yield
        if idx == 0:
            state["csum"] = None
        csum = state["csum"]
        if r >= 0:
            kb.op("gpsimd", lambda e: e.tensor_tensor(out=SP[:], in0=SP[:], in1=cm[:, r * 512:(r + 1) * 512],
                                                      op=ALU.mult), reads=[SP, cm], writes=[SP])
        kb.op("tensor", lambda e: e.matmul(B[:], lhsT=tri[:], rhs=SP[:], start=True,
                                           stop=(csum is None)), reads=[tri, SP], writes=[B])
        if csum is not None:
            kb.op("tensor", lambda e: e.matmul(B[:], lhsT=ones[:], rhs=csum[:],
                                               start=False, stop=True), reads=[ones, csum], pwrites=[B])
        if idx + 1 < n:
            cn = Cs[state["ci"] % 3]
            state["ci"] += 1
            if csum is None:
                kb.op("gpsimd", lambda e: e.tensor_copy(out=cn[:], in_=SP[:]), reads=[SP], writes=[cn])
            else:
                kb.op("gpsimd", lambda e: e.tensor_tensor(out=cn[:], in0=csum[:], in1=SP[:], op=ALU.add),
                      reads=[csum, SP], writes=[cn])
            state["csum"] = cn
        yield
        kb.op("vector", lambda e: e.scalar_tensor_tensor(out=t[:], in0=Z[:], scalar=SCALE, in1=SP[:],
                                                         op0=ALU.mult, op1=ALU.subtract), reads=[Z, SP], writes=[t])
        kb.op("vector", lambda e: e.tensor_tensor(out=t[:], in0=t[:], in1=B[:], op=ALU.subtract), reads=[t, B],
              writes=[t])
        kb.op("scalar", lambda e: e.activation(out=Wt[:], in_=t[:], func=AF.Exp), reads=[t], writes=[Wt])
        yield
        if r >= 0:
            kb.op("gpsimd", lambda e: e.tensor_tensor(out=Wt[:], in0=Wt[:], in1=cm[:, r * 512:(r + 1) * 512],
                                                      op=ALU.mult), reads=[Wt, cm], writes=[Wt])
        kb.op("tensor", lambda e: e.matmul(O[:], lhsT=v[:, kt, :], rhs=Wt[:], start=(idx == 0), stop=(idx == n - 1)),
              reads=[v, Wt], writes=[O] if idx == 0 else (), pwrites=() if idx == 0 else [O])
        if idx == n - 1:
            P.evac(sg[:], O[:], reads=[O], writes=[sg])
            kb.dma("sync", P.MIX.t[h * 128:(h + 1) * 128, qb * 512:(qb + 1) * 512], sg[:], reads=[sg],
                   pwrites=[P.MIX], sem_of=sg)

    def all_pairs():
        c = 0
        oc = 0
        for hi, h in enumerate(heads):
            for qb in range(S // 512):
                kts = list(range(4 * qb + 3, -1, -1))
                for idx, kt in enumerate(kts):
                    yield pair_gen(hi, h, qb, kt, idx, len(kts), c, oc)
                    c += 1
                oc += 1

    run_pipelined(all_pairs())
    kb.end_phase()


def phase_softmax_attn(P, kind, nheads=8):
    kb = P.kb
    S, NT = P.S, P.NT
    kb.begin_phase()
    bufs = _attn_bufs(P)
    onesb = P.load_const("ones", [128, 128], BF16)
    zb = [kb.ps([128, 512], F32, "z") for _ in range(2)]
    ob = [kb.ps([128, 512], F32, "o") for _ in range(2)]
    db = [kb.ps([128, 512], F32, "dn") for _ in range(2)]
    NR = 3
    Pb = [kb.sb([128, 512], BF16, "P") for _ in range(NR)]
    rec = [kb.sb([128, 512], F32, "rec") for _ in range(2)]
    stg = [kb.sb([128, 512], BF16, "stg") for _ in range(2)]
    if kind == "moba":
        cm = P.load_const("cmaskb", [128, 4 * 512], BF16, src=None, q=None) if False else None
        cmb = kb.sb([128, 4 * 512], BF16, "cmb")
        kb.dma("gpsimd", cmb[:], P.cst["cmask"].t[:, 4 * 512:8 * 512], reads=[P.cst["cmask"]], writes=[cmb],
               sem_of=cmb)
        esel = P.load_const("esel", [128, 16 * 128], BF16)
        gmask = P.load_const("gmask", [128, NT * 16], F32)
        ownm = P.load_const("ownmask", [128, NT * 16], F32)
        identb = P.load_const("ident", [128, 128], BF16)
        gps = kb.ps([128, 512], F32, "gps")
        bps = kb.ps([128, 1024], BF16, "bps")
        kmf = kb.sb([128, 16], F32, "kmf")
        kmb = kb.sb([128, 16], BF16, "kmb")
        g = kb.sb([128, NT * 16], F32, "g")
        m8 = kb.sb([128, NT * 8], F32, "m8")
        sel = kb.sb([128, NT * 16], F32, "sel")
        biasb = kb.sb([128, NT * 128], BF16, "biasb")
        kb.op("gpsimd", lambda e: e.memset(biasb[:], 0.0), writes=[biasb])
        biasT = [kb.sb([128, S], BF16, "biasT") for _ in range(2)]
        qoff, koff, voff, ooff = 0, 1024, 0, 0
        nb = S // 256
    else:
        mfull = P.load_const("mfull", [128, 2944], BF16)
        zb = zb + [kb.ps([128, 512], F32, "z")]
        qoff, koff, voff, ooff = 2048, 3072, 1024, 1024
    heads = list(range(nheads))
    h0 = heads[0]
    hb = {0: _attn_load_head(P, bufs, 0, qoff + h0 * 128, koff + h0 * 128, voff + h0 * 128)}
    NR = 5
    Pb = Pb + [kb.sb([128, 512], BF16, "P"), kb.sb([128, 512], BF16, "P")]

    def prologue(hi):
        qT, kT, v = hb[hi]
        bT = biasT[hi % 2]
        kb.op("vector", lambda e: e.tensor_reduce(out=kmf[:, 0:nb], in_=kT.t[:, :].rearrange("p (n k) -> p n k", k=256),
                                                  axis=AX.X, op=ALU.add), reads=[kT], writes=[kmf])
        if nb < 16:
            kb.op("vector", lambda e: e.memset(kmf[:, nb:16], 0.0), pwrites=[kmf])
        kb.op("vector", lambda e: e.tensor_scalar(out=kmb[:], in0=kmf[:], scalar1=1.0 / 256, scalar2=None,
                                                  op0=ALU.mult), reads=[kmf], writes=[kmb])
        for t in range(NT):
            kb.op("tensor", lambda e, t=t: e.matmul(gps[:, t * 16:(t + 1) * 16], lhsT=qT[:, t * 128:(t + 1) * 128],
                                                     rhs=kmb[:], start=True, stop=True),
                  reads=[qT, kmb], writes=[gps] if t == 0 else (), pwrites=() if t == 0 else [gps])
        kb.op("vector", lambda e: e.tensor_tensor(out=g[:], in0=gps[:, 0:NT * 16], in1=gmask[:], op=ALU.add),
              reads=[gps, gmask], writes=[g])
        for t in range(NT):
            kb.op("vector", lambda e, t=t: e.max(out=m8[:, t * 8:(t + 1) * 8], in_=g[:, t * 16:(t + 1) * 16]),
                  reads=[g], pwrites=[m8])
        for t in range(NT):
            kb.op("vector", lambda e, t=t: e.tensor_scalar(out=sel[:, t * 16:(t + 1) * 16], in0=g[:, t * 16:(t + 1) * 16],
                                                            scalar1=m8[:, t * 8 + 2:t * 8 + 3], scalar2=None,
                                                            op0=ALU.is_ge), reads=[g, m8], pwrites=[sel])
        kb.op("vector", lambda e: e.tensor_tensor(out=sel[:], in0=sel[:], in1=ownm[:], op=ALU.max),
              reads=[sel, ownm], writes=[sel])
        kb.op("vector", lambda e: e.tensor_scalar(
            out=biasb.t[:, :].rearrange("p (t c) -> p t c", c=128)[:, :, 0:16],
            in0=sel.t[:, :].rearrange("p (t c) -> p t c", c=16), scalar1=-1.0, scalar2=-NEG,
            op0=ALU.add, op1=ALU.mult), reads=[sel], writes=[biasb])
        for t0 in range(0, NT, 8):
            nt8 = min(8, NT - t0)
            for t in range(t0, t0 + nt8):
                kb.op("tensor", lambda e, t=t: e.transpose(bps[:, (t - t0) * 128:(t - t0 + 1) * 128],
                                                            biasb[:, t * 128:(t + 1) * 128], identb[:]),
                      reads=[biasb, identb], writes=[bps] if t == t0 else (), pwrites=() if t == t0 else [bps])
            P.evac(bT[:, t0 * 128:(t0 + nt8) * 128], bps[:, 0:nt8 * 128], reads=[bps], pwrites=[bT])

    pairs = []
    c = 0
    oc = 0
    for hi, h in enumerate(heads):
        for qb in range(S // 512):
            if kind == "moba":
                kts = list(range(0, 4 * qb + 4))
            else:
                kts = list(range(max(0, 4 * qb - 16), 4 * qb + 4))
            for idx, kt in enumerate(kts):
                pairs.append(dict(hi=hi, h=h, qb=qb, kt=kt, idx=idx, n=len(kts), c=c, oc=oc,
                                  first_of_head=(qb == 0 and idx == 0), second_of_head=(qb == 0 and idx == 1)))
                c += 1
            oc += 1
    NZ = len(zb)

    def pair_gen(p):
        hi, h, qb, kt, idx, c = p["hi"], p["h"], p["qb"], p["kt"], p["idx"], p["c"]
        if qb == 1 and idx == 0 and hi + 1 < len(heads):
            h2 = heads[hi + 1]
            hb[hi + 1] = _attn_load_head(P, bufs, hi + 1, qoff + h2 * 128, koff + h2 * 128, voff + h2 * 128)
        if p["first_of_head"] and kind == "moba":
            prologue(hi)
        qT, kT, v = hb[hi]
        r = kt - 4 * qb
        Z = zb[c % NZ]
        Pt = Pb[c % NR]
        O, Dn = ob[p["oc"] % 2], db[p["oc"] % 2]
        sg, rc = stg[p["oc"] % 2], rec[p["oc"] % 2]
        first, last = idx == 0, idx == p["n"] - 1
        if kind == "moba":
            bT = biasT[hi % 2]
            n = kt // 2
            kb.op("tensor", lambda e: e.matmul(Z[:], lhsT=kT[:, kt * 128:(kt + 1) * 128],
                                               rhs=qT[:, qb * 512:(qb + 1) * 512], start=True, stop=False),
                  reads=[kT, qT], writes=[Z])
            kb.op("tensor", lambda e: e.matmul(Z[:], lhsT=esel[:, n * 128:(n + 1) * 128],
                                               rhs=bT[:, qb * 512:(qb + 1) * 512], start=False, stop=True),
                  reads=[esel, bT], pwrites=[Z])
        else:
            kb.op("tensor", lambda e: e.matmul(Z[:], lhsT=kT[:, kt * 128:(kt + 1) * 128],
                                               rhs=qT[:, qb * 512:(qb + 1) * 512], start=True, stop=True),
                  reads=[kT, qT], writes=[Z])
        yield
        kb.op("scalar", lambda e: e.activation(out=Pt[:], in_=Z[:], func=AF.Exp, scale=SCALE), reads=[Z], writes=[Pt])
        if kind == "moba":
            if r >= 0:
                kb.op("vector", lambda e: e.tensor_tensor(out=Pt[:], in0=Pt[:], in1=cmb[:, r * 512:(r + 1) * 512],
                                                          op=ALU.mult), reads=[Pt, cmb], writes=[Pt])
        else:
            x0 = (qb * 512 - kt * 128) + 384
            eng = "vector" if c % 2 == 0 else "gpsimd"
            kb.op(eng, lambda e: e.tensor_tensor(out=Pt[:], in0=Pt[:], in1=mfull[:, x0:x0 + 512], op=ALU.mult),
                  reads=[Pt, mfull], writes=[Pt])
        yield
        kb.op("tensor", lambda e: e.matmul(O[:], lhsT=v[:, kt, :], rhs=Pt[:], start=first, stop=last),
              reads=[v, Pt], writes=[O] if first else (), pwrites=() if first else [O])
        kb.op("tensor", lambda e: e.matmul(Dn[:], lhsT=onesb[:], rhs=Pt[:], start=first, stop=last),
              reads=[onesb, Pt], writes=[Dn] if first else (), pwrites=() if first else [Dn])
        if last:
            kb.op("vector", lambda e: e.reciprocal(out=rc[:], in_=Dn[:]), reads=[Dn], writes=[rc])
            kb.op("vector", lambda e: e.tensor_tensor(out=sg[:], in0=rc[:], in1=O[:], op=ALU.mult), reads=[rc, O],
                  writes=[sg])
            r0 = ooff + h * 128
            kb.dma("sync", P.MIX.t[r0:r0 + 128, qb * 512:(qb + 1) * 512], sg[:], reads=[sg], pwrites=[P.MIX],
                   sem_of=sg)

    run_pipelined(pair_gen(p) for p in pairs)
    kb.end_phase()


def phase_gdn(P, nheads=8):
    import os
    STOP = int(os.environ.get("GDN_STOP", "99"))
    H5S = int(os.environ.get("H5_STOP", "99"))
    kb = P.kb
    S, NT = P.S, P.NT
    NC = S // 64
    kb.begin_phase()
    pz = [kb.ps([128, 512], F32, "pz") for _ in range(6)]
    pzb = [kb.ps([128, 1024], BF16, "pzb") for _ in range(2)]
    identf = P.load_const("ident", [128, 128], F32)
    identb = P.load_const("ident", [128, 128], BF16)
    onesb = P.load_const("ones", [128, 128], BF16)
    sel8 = P.load_const("sel8", [128, 8 * 128], F32)
    negst = P.load_const("negstrict", [128, 128], F32)
    neginc = P.load_const("negincT", [128, 128], F32)
    rowm = P.load_const("rowmask", [128, 2], F32)
    mh = kb.sb([128, 1], F32, "mhalf")
    kb.op("gpsimd", lambda e: e.memset(mh[:], -0.5), writes=[mh])

    gam = kb.sb([128, S], F32, "gam")
    gamT = kb.sb([128, NT * 8], F32, "gamT")
    glT = kb.sb([128, NT * 8], F32, "glT")
    eg = kb.sb([128, NT * 8], F32, "eg")
    kdf = kb.sb([128, NT * 8], F32, "kdf")
    beta = kb.sb([128, NT * 8], F32, "beta")
    nbeta = kb.sb([128, NT * 8], F32, "nbeta")
    beg = kb.sb([128, NT * 8], F32, "beg")
    egm = kb.sb([128, 2, NT * 8], F32, "egm")
    decS = kb.sb([128, 8, NC], F32, "decS")
    cw = kb.sb([128, 24, 4], F32, "cw")
    onw = kb.sb([128, 128], F32, "onw")
    kb.push_scope()
    scanm = P.load_const("scanmask", [128, S], F32)
    tmpA = kb.sb([128, S], F32, "tmpA")
    tmpB = kb.sb([128, S], F32, "tmpB")
    kb.op("vector", lambda e: e.memset(tmpA[:], 0.0), writes=[tmpA])
    kb.dma("sync", tmpA[0:8, :], P.AFM[:, :], reads=[P.AFM], writes=[tmpA], sem_of=tmpA)
    sc8 = kb.sb([128, 4], F32, "sc8")
    kb.op("vector", lambda e: e.memset(sc8[:], 0.0), writes=[sc8])
    kb.dma("sync", sc8[0:8, 0:1], P.din["ev_a_log"].t.rearrange("o h -> h o"), reads=[P.din["ev_a_log"]],
           writes=[sc8], sem_of=sc8)
    kb.dma("sync", sc8[0:8, 1:2], P.din["ev_dt_bias"].t.rearrange("o h -> h o"), reads=[P.din["ev_dt_bias"]],
           writes=[sc8], sem_of=sc8)
    kb.op("scalar", lambda e: e.activation(out=sc8[:, 2:3], in_=sc8[:, 0:1], func=AF.Exp), reads=[sc8], pwrites=[sc8])
    kb.op("vector", lambda e: e.tensor_scalar(out=sc8[:, 3:4], in0=sc8[:, 2:3], scalar1=-1.0, scalar2=None,
                                              op0=ALU.mult), reads=[sc8], pwrites=[sc8])
    kb.op("scalar", lambda e: e.activation(out=tmpB[:], in_=tmpA[:], func=AF.Exp, bias=sc8[:, 1:2], scale=1.0),
          reads=[tmpA, sc8], writes=[tmpB])
    kb.op("scalar", lambda e: e.activation(out=tmpB[:], in_=tmpB[:], func=AF.Ln, bias=1.0, scale=1.0),
          reads=[tmpB], writes=[tmpB])
    kb.op("vector", lambda e: e.tensor_scalar(out=tmpB[:], in0=tmpB[:], scalar1=sc8[:, 3:4], scalar2=None,
                                              op0=ALU.mult), reads=[tmpB, sc8], writes=[tmpB])
    kb.op("vector", lambda e: e.tensor_tensor_scan(out=gam[:], data0=scanm[:], data1=tmpB[:], initial=0.0,
                                                   op0=ALU.mult, op1=ALU.add), reads=[scanm, tmpB], writes=[gam])
    gv = gam.t[:, :].rearrange("p (n c) -> p n c", c=64)
    kb.op("vector", lambda e: e.tensor_copy(out=tmpA.t[:, :].rearrange("p (n c) -> p n c", c=64),
                                            in_=gv[:, :, 63:64].to_broadcast([128, NC, 64])),
          reads=[gam], writes=[tmpA])
    for (src, dst) in ((gam, gamT), (tmpA, glT)):
        for t0 in range(0, NT, 4):
            pp = pz[(t0 // 4) % 2]
            for t in range(t0, t0 + 4):
                kb.op("tensor", lambda e, t=t: e.transpose(pp[:, (t - t0) * 128:(t - t0 + 1) * 128],
                                                            src[:, t * 128:(t + 1) * 128], identf[:]),
                      reads=[src, identf], writes=[pp] if t == t0 else (), pwrites=() if t == t0 else [pp])
            P.evac(dst.t[:, t0 * 8:(t0 + 4) * 8].rearrange("p (t h) -> p t h", h=8),
                   pp.t[:, :].rearrange("p (t c) -> p t c", c=128)[:, :, 0:8], reads=[pp], pwrites=[dst])
    kb.op("scalar", lambda e: e.activation(out=eg[:], in_=gamT[:], func=AF.Exp), reads=[gamT], writes=[eg])
    kb.op("vector", lambda e: e.tensor_tensor(out=kdf[:], in0=glT[:], in1=gamT[:], op=ALU.subtract),
          reads=[glT, gamT], writes=[kdf])
    kb.op("scalar", lambda e: e.activation(out=kdf[:], in_=kdf[:], func=AF.Exp), reads=[kdf], writes=[kdf])
    ab = kb.sb([128, NT, 16], F32, "ab")
    kb.dma("sync", ab[:], P.ABtm.t[:, :].rearrange("(t p) c -> p t c", p=128), reads=[P.ABtm], writes=[ab], sem_of=ab)
    kb.op("scalar", lambda e: e.activation(out=beta.t[:, :].rearrange("p (t h) -> p t h", h=8), in_=ab[:, :, 8:16],
                                           func=AF.Sigmoid), reads=[ab], writes=[beta])
    kb.op("vector", lambda e: e.tensor_scalar(out=nbeta[:], in0=beta[:], scalar1=-1.0, scalar2=None, op0=ALU.mult),
          reads=[beta], writes=[nbeta])
    kb.op("vector", lambda e: e.tensor_tensor(out=beg[:], in0=beta[:], in1=eg[:], op=ALU.mult),
          reads=[beta, eg], writes=[beg])
    for hf in range(2):
        kb.op("vector", lambda e, hf=hf: e.tensor_scalar(out=egm[:, hf, :], in0=eg[:], scalar1=rowm[:, hf:hf + 1],
                                                          scalar2=None, op0=ALU.mult), reads=[eg, rowm], pwrites=[egm])
    glc = kb.sb([128, NC], F32, "glc")
    kb.op("vector", lambda e: e.tensor_copy(out=glc[:], in_=gv[:, :, 63]), reads=[gam], writes=[glc])
    for h in range(8):
        pp = pz[h % 2]
        kb.op("tensor", lambda e: e.matmul(pp[:, 0:NC], lhsT=sel8[:, h * 128:(h + 1) * 128], rhs=glc[:], start=True,
                                           stop=True), reads=[sel8, glc], writes=[pp])
        kb.op("scalar", lambda e: e.activation(out=decS[:, h, :], in_=pp[:, 0:NC], func=AF.Exp), reads=[pp],
              pwrites=[decS])
    cw4 = kb.sb([128, 3072], F32, "cw4")
    kb.op("gpsimd", lambda e: e.memset(cw4[:], 0.0), writes=[cw4])
    kb.dma("sync", cw4[0:4, :], P.din["ev_conv_w"][:, :], reads=[P.din["ev_conv_w"]], writes=[cw4], sem_of=cw4)
    for t0 in range(0, 24, 4):
        pp = pz[(t0 // 4) % 2]
        for t in range(t0, t0 + 4):
            kb.op("tensor", lambda e, t=t: e.transpose(pp[:, (t - t0) * 128:(t - t0 + 1) * 128],
                                                        cw4[:, t * 128:(t + 1) * 128], identf[:]),
                  reads=[cw4, identf], writes=[pp] if t == t0 else (), pwrites=() if t == t0 else [pp])
        P.evac(cw[:, t0:t0 + 4, :], pp.t[:, :].rearrange("p (t c) -> p t c", c=128)[:, :, 0:4], reads=[pp], pwrites=[cw])
    kb.dma("sync", onw[:], P.din["ev_onorm"].t[0:1, :].to_broadcast([128, 128]), reads=[P.din["ev_onorm"]],
           writes=[onw], sem_of=onw)

    kb.pop_scope()
    if STOP == 0:
        kb.end_phase()
        return
    xin = [kb.sb([128, S], BF16, "xin") for _ in range(2)]
    acc = kb.sb([128, S], F32, "acc")
    qnT = kb.sb([128, S], BF16, "qnT")
    knT = kb.sb([128, S], BF16, "knT")
    vcT = kb.sb([128, S], BF16, "vcT")
    kn_tm = kb.sb([128, NT, 128], BF16, "kn_tm")
    v_tm = kb.sb([128, NT, 128], BF16, "v_tm")
    z_tm = kb.sb([128, NT, 128], BF16, "z_tm")
    kd = kb.sb([128, NT, 128], BF16, "kd")
    ITb = kb.sb([128, NT, 128], BF16, "IT")
    ub = kb.sb([128, NT, 128], F32, "u")
    wT0 = kb.sb([128, NT, 128], BF16, "wT0")
    wT1 = kb.sb([128, NT, 128], BF16, "wT1")
    kb.op("gpsimd", lambda e: e.memset(wT0[:], 0.0), writes=[wT0])
    kb.op("gpsimd", lambda e: e.memset(wT1[:], 0.0), writes=[wT1])
    oacc = kb.sb([128, NT, 128], F32, "oacc")
    sq = [kb.sb([128, 512], BF16, "sq") for _ in range(2)]
    rr = [kb.sb([128, 512], F32, "rr") for _ in range(2)]
    LANES = 3
    lanes = []
    for L in range(LANES):
        ln = dict(kg=kb.sb([128, 128], BF16, "kbg"), vb=kb.sb([128, 128], BF16, "vb"),
                  a1=kb.sb([128, 128], F32, "e1"), a2=kb.sb([128, 128], F32, "e2"),
                  N=[kb.sb([128, 128], F32, "N") for _ in range(2)],
                  NT=[kb.sb([128, 128], F32, "NTm") for _ in range(2)],
                  P=[kb.sb([128, 128], F32, "Pm") for _ in range(2)],
                  MT=kb.sb([128, 128], BF16, "MT"))
        ln["bankA"] = pz[2 * L]
        ln["bankB"] = pz[2 * L + 1]
        lanes.append(ln)
    Sf = kb.sb([128, 128], F32, "Sf")
    Sb = kb.sb([128, 128], BF16, "Sb")
    vnew = [kb.sb([128, 128], BF16, "vnew") for _ in range(2)]
    ot = [kb.sb([128, 128], F32, "ot") for _ in range(2)]
    ot2 = [kb.sb([128, 128], F32, "ot2") for _ in range(2)]
    ss = [kb.sb([128, 2], F32, "ss") for _ in range(2)]
    junk = kb.sb([128, 128], BF16, "junk")
    yo = [kb.sb([128, 128], F32, "yo") for _ in range(2)]
    zs = [kb.sb([128, 128], F32, "zs") for _ in range(2)]
    yb = [kb.sb([128, 128], BF16, "ybo") for _ in range(2)]
    stg = [kb.sb([128, 512], BF16, "stg") for _ in range(2)]
    LNQ = float(np.log(HD ** -0.5))
    xi = 0
    for h in range(nheads):
        kb.dma("sync", z_tm[:], P.TM.t[:, 1024 + h * 128:1024 + (h + 1) * 128].rearrange("(t p) d -> p t d", p=128),
               reads=[P.TM], writes=[z_tm], sem_of=z_tm)
        for gi, dstT in enumerate((qnT, knT, vcT)):
            x = xin[xi % 2]
            xi += 1
            ct = gi * 8 + h
            r0 = 2048 + ct * 128
            kb.dma("sync", x[:], P.FM.t[r0:r0 + 128, :], reads=[P.FM], writes=[x], sem_of=x)
            kb.op("vector", lambda e: e.tensor_scalar(out=acc[:], in0=x[:], scalar1=cw[:, ct, 3:4], scalar2=None,
                                                      op0=ALU.mult), reads=[x, cw], writes=[acc])
            for sft in (1, 2, 3):
                kb.op("vector", lambda e, sft=sft: e.scalar_tensor_tensor(
                    out=acc[:, sft:S], in0=x[:, 0:S - sft], scalar=cw[:, ct, 3 - sft:4 - sft], in1=acc[:, sft:S],
                    op0=ALU.mult, op1=ALU.add), reads=[x, cw, acc], writes=[acc])
            kb.op("scalar", lambda e: e.activation(out=dstT[:], in_=acc[:], func=AF.Silu), reads=[acc], writes=[dstT])
        if STOP == 1:
            continue
        bi = 0
        for (xT, lnb) in ((qnT, LNQ), (knT, 0.0)):
            for blk in range(S // 512):
                s_, pp = sq[bi % 2], pz[bi % 2]
                bi += 1
                sl = slice(blk * 512, (blk + 1) * 512)
                kb.op("gpsimd", lambda e: e.tensor_tensor(out=s_[:], in0=xT[:, sl], in1=xT[:, sl], op=ALU.mult),
                      reads=[xT], writes=[s_])
                kb.op("tensor", lambda e: e.matmul(pp[:], lhsT=onesb[:], rhs=s_[:], start=True, stop=True),
                      reads=[onesb, s_], writes=[pp])
                kb.op("scalar", lambda e: e.activation(out=acc[:, sl], in_=pp[:], func=AF.Ln, bias=EPS, scale=1.0),
                      reads=[pp], writes=[acc] if blk == 0 else (), pwrites=() if blk == 0 else [acc])
            kb.op("scalar", lambda e: e.activation(out=acc[:], in_=acc[:], func=AF.Exp, bias=lnb, scale=-0.5),
                  reads=[acc], writes=[acc])
            kb.op("vector", lambda e: e.tensor_tensor(out=xT[:], in0=xT[:], in1=acc[:], op=ALU.mult),
                  reads=[xT, acc], writes=[xT])
        if STOP == 2:
            continue
        for (srcT, dst) in ((knT, kn_tm), (vcT, v_tm)):
            for t0 in range(0, NT, 8):
                pp = pzb[(t0 // 8) % 2]
                n8 = min(8, NT - t0)
                for t in range(t0, t0 + n8):
                    kb.op("tensor", lambda e, t=t: e.transpose(pp[:, (t - t0) * 128:(t - t0 + 1) * 128],
                                                                srcT[:, t * 128:(t + 1) * 128], identb[:]),
                          reads=[srcT, identb], writes=[pp] if t == t0 else (), pwrites=() if t == t0 else [pp])
                P.evac(dst.t[:, t0:t0 + n8, :], pp.t[:, 0:n8 * 128].rearrange("p (t c) -> p t c", c=128), reads=[pp],
                       pwrites=[dst])
        if STOP == 3:
            continue
        kb.barrier()

        def tile_gen(i, L, h=h):
            col = i * 8 + h
            tl = slice(i * 128, (i + 1) * 128)
            ln = lanes[L]
            kg, vb_, a1, a2, MT = ln["kg"], ln["vb"], ln["a1"], ln["a2"], ln["MT"]
            bA, bB = ln["bankA"], ln["bankB"]
            A0, A1 = bA.t[:, 0:128], bA.t[:, 128:256]
            B0, B1, B2 = bB.t[:, 0:128], bB.t[:, 128:256], bB.t[:, 256:384]
            kb.op("gpsimd", lambda e: e.tensor_scalar(out=kg[:], in0=kn_tm[:, i, :], scalar1=beg[:, col:col + 1],
                                                      scalar2=None, op0=ALU.mult), reads=[kn_tm, beg], writes=[kg])
            kb.op("gpsimd", lambda e: e.tensor_scalar(out=vb_[:], in0=v_tm[:, i, :], scalar1=beta[:, col:col + 1],
                                                      scalar2=None, op0=ALU.mult), reads=[v_tm, beta], writes=[vb_])
            kb.op("gpsimd", lambda e: e.tensor_scalar(out=kd[:, i, :], in0=kn_tm[:, i, :], scalar1=kdf[:, col:col + 1],
                                                      scalar2=None, op0=ALU.mult), reads=[kn_tm, kdf], pwrites=[kd])
            kb.op("tensor", lambda e: e.matmul(B0, lhsT=knT[:, tl], rhs=knT[:, tl], start=True, stop=True),
                  reads=[knT], writes=[bB])
            kb.op("tensor", lambda e: e.matmul(B1, lhsT=knT[:, tl], rhs=qnT[:, tl], start=True, stop=True),
                  reads=[knT, qnT], writes=[bB])
            kb.op("tensor", lambda e: e.matmul(B2, lhsT=sel8[:, h * 128:(h + 1) * 128], rhs=gam[:, tl],
                                               start=True, stop=True), reads=[sel8, gam], writes=[bB])
            yield
            kb.op("vector", lambda e: e.tensor_scalar(out=a1[:], in0=B2, scalar1=gamT[:, col:col + 1],
                                                      scalar2=None, op0=ALU.subtract), reads=[bB, gamT], writes=[a1])
            kb.op("vector", lambda e: e.tensor_scalar(out=a2[:], in0=B2, scalar1=gamT[:, col:col + 1],
                                                      scalar2=None, op0=ALU.subtract), reads=[bB, gamT], writes=[a2])
            yield
            kb.op("vector", lambda e: e.scalar_tensor_tensor(out=a1[:], in0=a1[:], scalar=0.0, in1=negst[:],
                                                             op0=ALU.max, op1=ALU.subtract), reads=[a1, negst],
                  writes=[a1])
            kb.op("vector", lambda e: e.scalar_tensor_tensor(out=a2[:], in0=a2[:], scalar=0.0, in1=neginc[:],
                                                             op0=ALU.min, op1=ALU.add), reads=[a2, neginc], writes=[a2])
            yield
            kb.op("scalar", lambda e: e.activation(out=a1[:], in_=a1[:], func=AF.Exp, scale=-1.0), reads=[a1],
                  writes=[a1])
            kb.op("scalar", lambda e: e.activation(out=a2[:], in_=a2[:], func=AF.Exp), reads=[a2], writes=[a2])
            yield
            NTk, Nk = ln["NT"][0], ln["N"][0]
            kb.op("vector", lambda e: e.scalar_tensor_tensor(out=NTk[:], in0=B0, scalar=nbeta[:, col:col + 1],
                                                             in1=a1[:], op0=ALU.mult, op1=ALU.mult),
                  reads=[bB, nbeta, a1], writes=[NTk])
            kb.op("vector", lambda e: e.tensor_tensor(out=ITb[:, i, :], in0=a2[:], in1=B1, op=ALU.mult),
                  reads=[a2, bB], pwrites=[ITb])
            yield
            kb.op("tensor", lambda e: e.transpose(A1, NTk[:], identf[:]), reads=[NTk, identf], writes=[bA])
            yield
            kb.op("scalar", lambda e: e.activation(out=Nk[:], in_=A1, func=AF.Copy), reads=[bA], writes=[Nk])
            yield
            Pm = ln["P"][0]
            kb.op("gpsimd", lambda e: e.tensor_tensor(out=Pm[:], in0=identf[:], in1=Nk[:], op=ALU.add),
                  reads=[identf, Nk], writes=[Pm])
            for lvl in range(5):
                last = lvl == 4
                NT2, N2 = ln["NT"][(lvl + 1) % 2], ln["N"][(lvl + 1) % 2]
                kb.op("tensor", lambda e: e.matmul(A0, lhsT=Nk[:], rhs=NTk[:], start=True, stop=True),
                      reads=[Nk, NTk], writes=[bA])
                if not last:
                    kb.op("tensor", lambda e: e.matmul(B0, lhsT=NTk[:], rhs=Nk[:], start=True, stop=True),
                          reads=[Nk, NTk], writes=[bB])
                yield
                kb.op("scalar", lambda e: e.activation(out=NT2[:], in_=A0, func=AF.Copy), reads=[bA], writes=[NT2])
                if not last:
                    kb.op("vector", lambda e: e.tensor_copy(out=N2[:], in_=B0), reads=[bB], writes=[N2])
                yield
                kb.op("tensor", lambda e: e.matmul(B2, lhsT=NT2[:], rhs=Pm[:], start=True, stop=True),
                      reads=[NT2, Pm], writes=[bB])
                yield
                if last:
                    kb.op("vector", lambda e: e.tensor_tensor(out=MT[:], in0=Pm[:], in1=B2, op=ALU.add),
                          reads=[Pm, bB], writes=[MT])
                else:
                    Pn = ln["P"][(lvl + 1) % 2]
                    kb.op("vector", lambda e: e.tensor_tensor(out=Pn[:], in0=Pm[:], in1=B2, op=ALU.add),
                          reads=[Pm, bB], writes=[Pn])
                    Pm = Pn
                NTk, Nk = NT2, N2
            yield
            kb.op("tensor", lambda e: e.matmul(A0, lhsT=MT[:], rhs=vb_[:], start=True, stop=True),
                  reads=[MT, vb_], writes=[bA])
            kb.op("tensor", lambda e: e.matmul(B1, lhsT=kg[:], rhs=MT[:], start=True, stop=True),
                  reads=[MT, kg], writes=[bB])
            yield
            kb.op("scalar", lambda e: e.activation(out=ub[:, i, :], in_=A0, func=AF.Copy), reads=[bA], pwrites=[ub])
            kb.op("vector", lambda e: e.tensor_copy(out=wT0[:, i, 0:64], in_=B1[:, 0:64]), reads=[bB], pwrites=[wT0])
            kb.op("vector", lambda e: e.tensor_copy(out=wT1[:, i, 64:128], in_=B1[:, 64:128]), reads=[bB],
                  pwrites=[wT1])

        free_lanes = list(range(LANES))
        active = []
        ti = 0
        while ti < NT or active:
            while ti < NT and free_lanes:
                L = free_lanes.pop(0)
                active.append((tile_gen(ti, L), L))
                ti += 1
            nxt = []
            for g, L in active:
                try:
                    next(g)
                    nxt.append((g, L))
                except StopIteration:
                    free_lanes.append(L)
            active = nxt
        kb.barrier()
        if STOP == 4:
            continue
        kb.op("vector", lambda e: e.memset(Sf[:], 0.0), writes=[Sf])
        kb.op("vector", lambda e: e.memset(Sb[:], 0.0), writes=[Sb])
        for n in range(NC):
            i, hf = n // 2, n % 2
            col = i * 8 + h
            tl = slice(i * 128, (i + 1) * 128)
            wTp = wT0 if hf == 0 else wT1
            pWS, pO1, pO2, pSU = pz[0 + 4 * 0], pz[1], pz[2], pz[3]
            vn, o1, o2 = vnew[n % 2], ot[n % 2], ot2[n % 2]
            kb.op("tensor", lambda e: e.matmul(pWS[:, 0:128], lhsT=wTp[:, i, :], rhs=Sb[:], start=True, stop=True),
                  reads=[wTp, Sb], writes=[pWS])
            kb.op("tensor", lambda e: e.matmul(pO1[:, 0:128], lhsT=qnT[:, tl], rhs=Sb[:], start=True, stop=True),
                  reads=[qnT, Sb], writes=[pO1])
            kb.op("vector", lambda e: e.scalar_tensor_tensor(out=vn[:], in0=ub[:, i, :], scalar=rowm[:, hf:hf + 1],
                                                             in1=pWS[:, 0:128], op0=ALU.mult, op1=ALU.subtract),
                  reads=[ub, rowm, pWS], writes=[vn])
            kb.op("tensor", lambda e: e.matmul(pSU[:, 0:128], lhsT=kd[:, i, :], rhs=vn[:], start=True, stop=True),
                  reads=[kd, vn], writes=[pSU])
            kb.op("tensor", lambda e: e.matmul(pO2[:, 0:128], lhsT=ITb[:, i, :], rhs=vn[:], start=True, stop=True),
                  reads=[ITb, vn], writes=[pO2])
            kb.op("vector", lambda e: e.scalar_tensor_tensor(out=Sf[:], in0=Sf[:], scalar=decS[:, h, n:n + 1],
                                                             in1=pSU[:, 0:128], op0=ALU.mult, op1=ALU.add),
                  reads=[Sf, decS, pSU], writes=[Sf])
            kb.op("scalar", lambda e: e.activation(out=Sb[:], in_=Sf[:], func=AF.Copy), reads=[Sf], writes=[Sb])
            kb.op("vector", lambda e: e.tensor_scalar(out=o1[:], in0=pO1[:, 0:128], scalar1=egm[:, hf, col:col + 1],
                                                      scalar2=None, op0=ALU.mult), reads=[pO1, egm], writes=[o1])
            if hf == 0:
                kb.op("vector", lambda e: e.tensor_tensor(out=oacc[:, i, :], in0=o1[:], in1=pO2[:, 0:128], op=ALU.add),
                      reads=[o1, pO2], pwrites=[oacc])
            else:
                kb.op("vector", lambda e: e.tensor_tensor(out=o2[:], in0=o1[:], in1=pO2[:, 0:128], op=ALU.add),
                      reads=[o1, pO2], writes=[o2])
                kb.op("gpsimd", lambda e: e.tensor_tensor(out=oacc[:, i, :], in0=oacc[:, i, :], in1=o2[:], op=ALU.add),
                      reads=[oacc, o2], pwrites=[oacc])
        if STOP == 5:
            continue
        for i in range(NT):
            s_, y_, z_, yb_ = ss[i % 2], yo[i % 2], zs[i % 2], yb[i % 2]
            kb.op("scalar", lambda e: e.activation(out=junk[:], in_=oacc[:, i, :], func=AF.Square, accum_out=s_[:, 0:1]),
                  reads=[oacc], writes=[junk], pwrites=[s_])
            P.rstd_from_ss(s_, HD, mh)
            kb.op("vector", lambda e: e.scalar_tensor_tensor(out=y_[:], in0=oacc[:, i, :], scalar=s_[:, 1:2], in1=onw[:],
                                                             op0=ALU.mult, op1=ALU.mult), reads=[oacc, s_, onw],
                  writes=[y_])
            kb.op("scalar", lambda e: e.activation(out=z_[:], in_=z_tm[:, i, :], func=AF.Silu), reads=[z_tm], writes=[z_])
            kb.op("vector", lambda e: e.tensor_tensor(out=yb_[:], in0=y_[:], in1=z_[:], op=ALU.mult), reads=[y_, z_],
                  writes=[yb_])
            pp = pzb[(i // 4) % 2]
            j = i % 4
            kb.op("tensor", lambda e: e.transpose(pp[:, j * 128:(j + 1) * 128], yb_[:], identb[:]), reads=[yb_, identb],
                  writes=[pp] if j == 0 else (), pwrites=() if j == 0 else [pp])
            if j == 3:
                sg = stg[(i // 4) % 2]
                P.evac(sg[:], pp[:, 0:512], reads=[pp], writes=[sg])
                r0 = 1024 + h * 128
                t0 = (i // 4) * 512
                kb.dma("sync", P.MIX.t[r0:r0 + 128, t0:t0 + 512], sg[:], reads=[sg], pwrites=[P.MIX], sem_of=sg)
        kb.barrier()
    kb.end_phase()


def build_full(S):
    P = Prog(S)
    x = P.din["x"]
    P.phase_inproj(0, x)
    phase_sb(P)
    phase_gdn(P)
    P.phase_outproj_fused(0, P.din["ev_w_out"], x, P.H1)
    P.phase_gateup(0, P.H1)
    P.phase_gemm_tm(P.AT, FF, P.din["ffn_w_down"], 0, P.Y, 512)
    P.phase_norm_res(P.Y, P.H1, "ffn_norm_post", 0, P.H2)
    P.phase_inproj(1, P.H2)
    phase_softmax_attn(P, "moba")
    phase_softmax_attn(P, "dil")
    P.phase_outproj_fused(1, P.din["od_w_out"], P.H2, P.H3)
    P.phase_gateup(1, P.H3)
    P.phase_gemm_tm(P.AT, FF, P.din["ffn_w_down"], FF, P.Y, 512)
    P.phase_norm_res(P.Y, P.H3, "ffn_norm_post", 1, P.OUT)
    return P


def make_in_map(inputs, b, S, consts):
    m = {"x": np.ascontiguousarray(np.asarray(inputs["x"])[b, :S], dtype=np.float32)}
    for n, shp in WEIGHT_SPECS:
        m[n] = np.ascontiguousarray(np.asarray(inputs[n], dtype=np.float32).reshape(shp))
    for n, v in consts.items():
        m["c_" + n] = v
    return m


def kernel(**inputs):
    x = np.asarray(inputs["x"])
    B, S, _ = x.shape
    P = build_full(S)
    consts = make_consts(S)
    in_maps = [make_in_map(inputs, b, S, consts) for b in range(B)]
    res = run_bass_kernel_spmd(P.nc, in_maps, core_ids=list(range(B)))
    out = np.stack([np.asarray(res.results[b]["y"], dtype=np.float32) for b in range(B)], axis=0)
    return out
```
